# Optimizing a Trainium2 kernel written in Bass

```python
import math
import jax, jax.numpy as jnp
from jax import lax
import numpy as np

D_MODEL = 1024
BATCH = 8
SEQ = 4096
DEPTH = 4

N_META = 16
GRID_W = 64
ROPE_THETA = 10000.0
EPS = 1e-6
EXP_CLIP = 30.0

ATT_HEADS = 8
ATT_KV_HEADS = 2
HEAD_DIM = 64
Q_BLOCK = 128
ATT_WIDTH = ATT_HEADS * HEAD_DIM
KV_WIDTH = ATT_KV_HEADS * HEAD_DIM

HGRN_HEADS = 4
HGRN_EXPAND = 128
HGRN_HEAD_V = 128
HGRN_KEY_WIDTH = HGRN_HEADS * HGRN_EXPAND
HGRN_WIDTH = HGRN_HEADS * HGRN_HEAD_V

SSM_HEADS = 8
SSM_HEAD_DIM = 64
SSM_GROUPS = 2
SSM_STATE = 64
SSM_CONV = 7
SSM_WIDTH = SSM_HEADS * SSM_HEAD_DIM
SSM_CONV_DIM = SSM_WIDTH + 2 * SSM_GROUPS * SSM_STATE

CHUNK = 64
MIX_WIDTH = ATT_WIDTH + HGRN_WIDTH + SSM_WIDTH

IN_SIZES = (ATT_WIDTH, KV_WIDTH, KV_WIDTH,
            HGRN_KEY_WIDTH, HGRN_KEY_WIDTH, HGRN_KEY_WIDTH,
            HGRN_WIDTH, HGRN_WIDTH,
            SSM_WIDTH, SSM_CONV_DIM, 2 * SSM_HEADS)
IN_WIDTH = sum(IN_SIZES)

N_EXPERTS = 16
EXPERT_FF = 2048
CAPACITY_FACTOR = 2

kernel_name = 'hybrid_parallel_heads_ec_moe_encoder'


def _rmsnorm(x, w):
    xf = x.astype(jnp.float32)
    y = xf * lax.rsqrt(jnp.mean(xf * xf, axis=-1, keepdims=True) + EPS)
    return (y * w.astype(jnp.float32)).astype(x.dtype)


def _axial_rope_tables(n_tok):
    rows = n_tok // GRID_W
    row = jnp.repeat(jnp.arange(rows), GRID_W).astype(jnp.float32)
    col = (jnp.arange(rows * GRID_W) % GRID_W).astype(jnp.float32)
    n_pair = HEAD_DIM // 4
    inv = ROPE_THETA ** (-jnp.arange(n_pair, dtype=jnp.float32) / n_pair)
    ang = jnp.concatenate([row[:, None] * inv, col[:, None] * inv], axis=-1)
    ang = jnp.concatenate([jnp.zeros((N_META, HEAD_DIM // 2), jnp.float32), ang], axis=0)
    return jnp.cos(ang), jnp.sin(ang)


def _apply_rope(x, cos, sin):
    half = HEAD_DIM // 2
    x1, x2 = x[..., :half], x[..., half:]
    c, s = cos[None, :, None, :], sin[None, :, None, :]
    return jnp.concatenate([x1 * c - x2 * s, x1 * s + x2 * c], axis=-1).astype(x.dtype)


def _attention_group(q, k, v, q_norm_w, k_norm_w, cos, sin):
    bsz, L, _ = q.shape
    rep = ATT_HEADS // ATT_KV_HEADS
    q = _apply_rope(_rmsnorm(q.reshape(bsz, L, ATT_HEADS, HEAD_DIM), q_norm_w), cos, sin)
    k = _apply_rope(_rmsnorm(k.reshape(bsz, L, ATT_KV_HEADS, HEAD_DIM), k_norm_w), cos, sin)
    q = q.reshape(bsz, L, ATT_KV_HEADS, rep, HEAD_DIM).transpose(0, 2, 3, 1, 4)
    k = k.transpose(0, 2, 1, 3)
    v = v.reshape(bsz, L, ATT_KV_HEADS, HEAD_DIM).transpose(0, 2, 1, 3)
    scale = HEAD_DIM ** -0.5

    def attend(qb):
        s = jnp.einsum('bgrqd,bgkd->bgrqk', qb, k).astype(jnp.float32) * scale
        p = jax.nn.softmax(s, axis=-1).astype(v.dtype)
        return jnp.einsum('bgrqk,bgkd->bgrqd', p, v)

    o_meta = attend(q[:, :, :, :N_META])
    n_real = L - N_META
    n_blk = n_real // Q_BLOCK
    qr = q[:, :, :, N_META:].reshape(bsz, ATT_KV_HEADS, rep, n_blk, Q_BLOCK, HEAD_DIM)
    o_real = lax.map(attend, jnp.moveaxis(qr, 3, 0))
    o_real = jnp.moveaxis(o_real, 0, 3).reshape(bsz, ATT_KV_HEADS, rep, n_real, HEAD_DIM)
    o = jnp.concatenate([o_meta, o_real], axis=3)
    return o.transpose(0, 3, 1, 2, 4).reshape(bsz, L, ATT_WIDTH)


def _to_chunks(a):
    bsz, h, t = a.shape[:3]
    return jnp.moveaxis(a.reshape((bsz, h, t // CHUNK, CHUNK) + a.shape[3:]), 2, 0)


def _from_chunks(o):
    o = jnp.moveaxis(o, 0, 2)
    return o.reshape(o.shape[0], o.shape[1], -1, o.shape[-1])


def _masked_exp(mask, diff):
    return jnp.where(mask, jnp.exp(jnp.where(mask, diff, 0.0)), 0.0)


def _gla_chunk_scan(q, k, v, g):
    bsz, h, _, dk = q.shape
    dv = v.shape[-1]
    mask = jnp.tril(jnp.ones((CHUNK, CHUNK), bool))

    def step(S, inp):
        qi, ki, vi, gi = inp
        cum = jnp.cumsum(gi, axis=2)
        rel = _masked_exp(mask[:, :, None], cum[:, :, :, None, :] - cum[:, :, None, :, :])
        att = jnp.einsum('bhid,bhjd,bhijd->bhij', qi, ki, rel)
        o = jnp.einsum('bhij,bhjv->bhiv', att, vi) + jnp.einsum('bhid,bhdv->bhiv', qi * jnp.exp(cum), S)
        last = cum[:, :, -1:, :]
        S = jnp.exp(last[:, :, 0, :])[..., None] * S + jnp.einsum('bhjd,bhjv->bhdv', ki * jnp.exp(last - cum), vi)
        return S, o

    S0 = jnp.zeros((bsz, h, dk, dv), jnp.float32)
    _, o = lax.scan(step, S0, (_to_chunks(q), _to_chunks(k), _to_chunks(v), _to_chunks(g)))
    return _from_chunks(o)


def _ssd_chunk_scan(q, k, v, g):
    bsz, h, _, n = q.shape
    p = v.shape[-1]
    mask = jnp.tril(jnp.ones((CHUNK, CHUNK), bool))

    def step(S, inp):
        qi, ki, vi, gi = inp
        cum = jnp.cumsum(gi, axis=-1)
        decay = _masked_exp(mask, cum[..., :, None] - cum[..., None, :])
        att = jnp.einsum('bhin,bhjn->bhij', qi, ki) * decay
        o = jnp.einsum('bhij,bhjp->bhip', att, vi) + jnp.einsum('bhin,bhnp->bhip', qi, S) * jnp.exp(cum)[..., None]
        S = jnp.exp(cum[..., -1])[..., None, None] * S + jnp.einsum(
            'bhjn,bhjp->bhnp', ki * jnp.exp(cum[..., -1:] - cum)[..., None], vi)
        return S, o

    gc = jnp.moveaxis(g.reshape(bsz, h, -1, CHUNK), 2, 0)
    S0 = jnp.zeros((bsz, h, n, p), jnp.float32)
    _, o = lax.scan(step, S0, (_to_chunks(q), _to_chunks(k), _to_chunks(v), gc))
    return _from_chunks(o)


def _bidirectional(scan_fn, q, v, k_f, g_f, k_b, g_b):
    pad = (-N_META) % CHUNK

    def prep(a):
        return jnp.pad(a.astype(jnp.float32), [(0, 0), (0, 0), (pad, 0)] + [(0, 0)] * (a.ndim - 3))

    q, v, k_f, g_f, k_b, g_b = (prep(a) for a in (q, v, k_f, g_f, k_b, g_b))
    rev = lambda a: jnp.flip(a, axis=2)
    o = scan_fn(q, k_f, v, g_f) + rev(scan_fn(rev(q), rev(k_b), rev(v), rev(g_b)))
    return o[:, :, pad:]


def _hgrn2_group(q, f_fwd, f_bwd, i, gate, lb_fwd, lb_bwd, norm_w):
    bsz, L, _ = q.shape
    heads = lambda a, d: a.reshape(bsz, L, HGRN_HEADS, d).transpose(0, 2, 1, 3)

    def log_forget(f_pre, lb):
        xf = f_pre.astype(jnp.float32)
        return jax.nn.log_sigmoid(xf) + jnp.log1p(lb * jnp.exp(jnp.minimum(-xf, EXP_CLIP)))

    g_f = heads(jnp.minimum(log_forget(f_fwd, lb_fwd), 0.0), HGRN_EXPAND)
    g_b = heads(jnp.minimum(log_forget(f_bwd, lb_bwd), 0.0), HGRN_EXPAND)
    k_f, k_b = -jnp.expm1(g_f), -jnp.expm1(g_b)
    qh = heads(jax.nn.silu(q), HGRN_EXPAND)
    vh = heads(i, HGRN_HEAD_V)
    o = _bidirectional(_gla_chunk_scan, qh, vh, k_f, g_f, k_b, g_b)
    o = o.transpose(0, 2, 1, 3).astype(q.dtype)
    o = _rmsnorm(o, norm_w.reshape(HGRN_HEADS, HGRN_HEAD_V)).reshape(bsz, L, HGRN_WIDTH)
    return o * jax.nn.silu(gate)


def _mamba2_group(z, xbc, dt_pre, conv_w, conv_b, dt_bias, a_log, d_skip, norm_w):
    bsz, L, _ = z.shape
    half = SSM_CONV // 2
    xbc = lax.conv_general_dilated(xbc, conv_w.T[:, None, :], window_strides=(1,),
                                   padding=[(half, half)], dimension_numbers=('NWC', 'WIO', 'NWC'),
                                   feature_group_count=SSM_CONV_DIM) + conv_b
    xbc = jax.nn.silu(xbc)
    xs, bm, cm = jnp.split(xbc, [SSM_WIDTH, SSM_WIDTH + SSM_GROUPS * SSM_STATE], axis=-1)
    xh = xs.reshape(bsz, L, SSM_HEADS, SSM_HEAD_DIM).transpose(0, 2, 1, 3)
    rep = SSM_HEADS // SSM_GROUPS
    expand = lambda a: jnp.repeat(a.reshape(bsz, L, SSM_GROUPS, SSM_STATE), rep, axis=2).transpose(0, 2, 1, 3)
    bh, ch = expand(bm), expand(cm)
    dt = jax.nn.softplus(dt_pre.astype(jnp.float32).reshape(bsz, L, 2, SSM_HEADS) + dt_bias)
    dt = dt.transpose(2, 0, 3, 1)
    A = -jnp.exp(a_log.astype(jnp.float32))
    g = dt * A[:, None, :, None]
    y = _bidirectional(_ssd_chunk_scan, ch, xh, bh * dt[0][..., None], g[0], bh * dt[1][..., None], g[1])
    y = y + d_skip[:, None, None] * xh
    y = y.transpose(0, 2, 1, 3).reshape(bsz, L, SSM_WIDTH).astype(z.dtype)
    return _rmsnorm(y * jax.nn.silu(z), norm_w)


def _expert_choice_ffn(h, router_w, w_gate, w_up, w_down):
    bsz, L, d = h.shape
    cap = CAPACITY_FACTOR * L // N_EXPERTS
    aff = jax.nn.softmax(jnp.einsum('bld,de->ble', h, router_w).astype(jnp.float32), axis=-1)
    gate, idx = lax.top_k(jnp.swapaxes(aff, 1, 2), cap)
    xs = jax.vmap(lambda hb, ib: hb[ib])(h, idx)
    a = jnp.einsum('becd,edf->becf', xs, w_gate)
    u = jnp.einsum('becd,edf->becf', xs, w_up)
    out = jnp.einsum('becf,efd->becd', jax.nn.silu(a) * u, w_down)
    out = out * gate[..., None].astype(out.dtype)
    return jax.vmap(lambda ob, ib: jax.ops.segment_sum(ob.reshape(-1, d), ib.reshape(-1), num_segments=L))(out, idx)


def setup_inputs(seed: int = 0) -> dict:
    key = jax.random.key(seed)
    ks = jax.random.split(key, 24)
    f32 = jnp.float32
    nrm = lambda k, shape, scale: scale * jax.random.normal(k, shape, f32)
    gain = lambda k, shape: 1.0 + 0.02 * jax.random.normal(k, shape, f32)
    dt0 = jnp.exp(jax.random.uniform(ks[10], (DEPTH, 2, SSM_HEADS), f32, math.log(1e-3), math.log(1e-1)))
    return {
        'x': jax.random.normal(ks[0], (BATCH, SEQ, D_MODEL), f32),
        'meta_tokens': nrm(ks[1], (N_META, D_MODEL), 1.0),
        'norm1_w': gain(ks[2], (DEPTH, D_MODEL)),
        'w_in': nrm(ks[3], (DEPTH, D_MODEL, IN_WIDTH), D_MODEL ** -0.5),
        'q_norm_w': gain(ks[4], (DEPTH, HEAD_DIM)),
        'k_norm_w': gain(ks[5], (DEPTH, HEAD_DIM)),
        'attn_norm_w': gain(ks[6], (DEPTH, ATT_WIDTH)),
        'hgrn_lb': nrm(ks[7], (2, DEPTH, HGRN_KEY_WIDTH), 0.1),
        'hgrn_norm_w': gain(ks[8], (DEPTH, HGRN_WIDTH)),
        'conv_w': nrm(ks[9], (DEPTH, SSM_CONV_DIM, SSM_CONV), SSM_CONV ** -0.5),
        'conv_b': nrm(ks[11], (DEPTH, SSM_CONV_DIM), 0.01),
        'dt_bias': dt0 + jnp.log(-jnp.expm1(-dt0)),
        'a_log': jnp.log(jax.random.uniform(ks[12], (DEPTH, 2, SSM_HEADS), f32, 1.0, 16.0)),
        'd_skip': gain(ks[13], (DEPTH, SSM_HEADS)),
        'ssm_norm_w': gain(ks[14], (DEPTH, SSM_WIDTH)),
        'w_out': nrm(ks[15], (DEPTH, MIX_WIDTH, D_MODEL), MIX_WIDTH ** -0.5),
        'norm2_w': gain(ks[16], (DEPTH, D_MODEL)),
        'router_w': nrm(ks[17], (DEPTH, D_MODEL, N_EXPERTS), D_MODEL ** -0.5),
        'w_gate': nrm(ks[18], (DEPTH, N_EXPERTS, D_MODEL, EXPERT_FF), D_MODEL ** -0.5),
        'w_up': nrm(ks[19], (DEPTH, N_EXPERTS, D_MODEL, EXPERT_FF), D_MODEL ** -0.5),
        'w_down': nrm(ks[20], (DEPTH, N_EXPERTS, EXPERT_FF, D_MODEL), EXPERT_FF ** -0.5),
    }


def reference(x, meta_tokens, norm1_w, w_in, q_norm_w, k_norm_w, attn_norm_w, hgrn_lb, hgrn_norm_w,
              conv_w, conv_b, dt_bias, a_log, d_skip, ssm_norm_w, w_out, norm2_w, router_w,
              w_gate, w_up, w_down):
    bsz, n_tok, _ = x.shape
    h = jnp.concatenate([jnp.broadcast_to(meta_tokens[None].astype(x.dtype), (bsz, N_META, D_MODEL)), x], axis=1)
    cos, sin = _axial_rope_tables(n_tok)
    p = jax.nn.softmax(hgrn_lb.astype(jnp.float32), axis=1)
    lower_bounds = jnp.clip(jnp.cumsum(p, axis=1) - p[:, :1], 0.0, 1.0)
    split_points = np.cumsum(IN_SIZES)[:-1].tolist()
    for l in range(DEPTH):
        u = _rmsnorm(h, norm1_w[l])
        proj = jnp.einsum('bld,dn->bln', u, w_in[l])
        aq, ak, av, hq, hf, hb, hi, hg, sz, sxbc, sdt = jnp.split(proj, split_points, axis=-1)
        att = _attention_group(aq, ak, av, q_norm_w[l], k_norm_w[l], cos, sin)
        rec = _hgrn2_group(hq, hf, hb, hi, hg, lower_bounds[0, l], lower_bounds[1, l], hgrn_norm_w[l])
        ssm = _mamba2_group(sz, sxbc, sdt, conv_w[l], conv_b[l], dt_bias[l], a_log[l], d_skip[l], ssm_norm_w[l])
        mix = jnp.concatenate([_rmsnorm(att, attn_norm_w[l]), rec, ssm], axis=-1)
        h = h + jnp.einsum('blm,md->bld', mix, w_out[l]).astype(h.dtype)
        h = h + _expert_choice_ffn(_rmsnorm(h, norm2_w[l]), router_w[l], w_gate[l], w_up[l], w_down[l]).astype(h.dtype)
    return h[:, N_META:]
```

```python
import numpy as np
import concourse.bass as bass
import concourse.mybir as mybir
from concourse.bass_utils import run_bass_kernel_spmd

F32 = mybir.dt.float32
BF16 = mybir.dt.bfloat16
I32 = mybir.dt.int32
U32 = mybir.dt.uint32
AF = mybir.ActivationFunctionType
ALU = mybir.AluOpType
AX = mybir.AxisListType

D = 1024
SEQ = 4096
NMETA = 16
L = SEQ + NMETA
DEPTH = 4
EPS = 1e-6
INW = 4624
NEXP = 16
FF = 2048
CAP = 2 * L // NEXP
NT = 33
import os
STQ = os.environ.get("MK_STQ", "pool")
MOE_DEPTH = DEPTH


def tile_rows(t):
    return NMETA if t == 0 else 128


def tile_off(t):
    return 0 if t == 0 else NMETA + 128 * (t - 1)


class K:
    def __init__(self, ap, key):
        self.ap = ap
        self.key = key


WRITE_ARGS = ("out", "accum_out", "out_max", "out_indices")
PSUM_KEYS = set()


class Sched:
    def __init__(self, nc, n_dma=12):
        self.nc = nc
        self.eng = {"pe": nc.tensor, "act": nc.scalar, "dve": nc.vector, "pool": nc.gpsimd, "sp": nc.sync}
        self.sem = {}
        self.val = {}
        for e in ("pe", "act", "dve", "pool"):
            self.sem[e] = nc.semaphore("s_" + e).__enter__()
            self.val[e] = 0
        self.dq = {}
        self.qeng = {"sp": "sp", "pool": "pool", "act": "act", "poolw": "pool"}
        for q in ("sp", "pool", "act", "poolw"):
            ids = []
            for i in range(n_dma if q != "poolw" else 3):
                tid = "d_%s%d" % (q, i)
                self.sem[tid] = nc.semaphore(tid).__enter__()
                self.val[tid] = 0
                ids.append(tid)
            self.dq[q] = [ids, 0]
        self.seen = {e: {} for e in self.eng}
        self.res = {}
        self.ninst = 0
        for h in self.sem.values():
            nc.gpsimd.sem_clear(h)
        nc.all_engine_barrier()

    def finish(self):
        self.barrier()
        self.nc.all_engine_barrier()
        for h in self.sem.values():
            self.nc.gpsimd.sem_clear(h)
        self.nc.all_engine_barrier()

    @staticmethod
    def key(x):
        if isinstance(x, K):
            return x.key
        if isinstance(x, (str, tuple)):
            return x
        return x.name

    def _wait(self, e, tl, v):
        if e == "pe" and tl == "pe":
            return
        if self.seen[e].get(tl, 0) >= v:
            return
        self.eng[e].wait_ge(self.sem[tl], v)
        self.seen[e][tl] = v
        self.ninst += 1

    def _deps(self, e, reads, writes):
        for k in reads:
            r = self.res.get(k)
            if r is not None and r[0] is not None:
                self._wait(e, *r[0])
            if r is not None and k in PSUM_KEYS:
                for tl, v in r[1].items():
                    if tl != e:
                        self._wait(e, tl, v)
        for k in writes:
            r = self.res.get(k)
            if r is not None:
                if r[0] is not None:
                    self._wait(e, *r[0])
                for tl, v in r[1].items():
                    self._wait(e, tl, v)

    def _commit(self, tl, v, reads, writes):
        for k in reads:
            r = self.res.setdefault(k, [None, {}])
            if r[1].get(tl, 0) < v:
                r[1][tl] = v
        for k in writes:
            self.res[k] = [(tl, v), {}]

    def _split(self, kw, extra_r, extra_w):
        reads, writes, real = [], [], {}
        for name, v in kw.items():
            if isinstance(v, K):
                (writes if name in WRITE_ARGS else reads).append(v.key)
                real[name] = v.ap
            elif isinstance(v, bass.AP):
                (writes if name in WRITE_ARGS else reads).append(v.name)
                real[name] = v
            else:
                real[name] = v
        reads += [self.key(x) for x in extra_r]
        writes += [self.key(x) for x in extra_w]
        return reads, writes, real

    def op(self, e, name, _r=(), _w=(), **kw):
        reads, writes, real = self._split(kw, _r, _w)
        self._deps(e, reads, writes)
        ins = getattr(self.eng[e], name)(**real)
        self.val[e] += 1
        ins.then_inc(self.sem[e], 1)
        self._commit(e, self.val[e], reads, writes)
        self.ninst += 1
        return ins

    def dma(self, q, out, in_, _r=(), _w=(), indirect=False, **kw):
        reads, writes, real = self._split(dict(out=out, in_=in_), _r, _w)
        ids, pos = self.dq[q]
        tid = ids[pos % len(ids)]
        self.dq[q][1] += 1
        q = self.qeng[q]
        if self.val[tid] > 0:
            self._wait(q, tid, self.val[tid])
        self._deps(q, reads, writes)
        if indirect:
            ins = self.eng[q].indirect_dma_start(out=real["out"], in_=real["in_"], **kw)
        else:
            ins = self.eng[q].dma_start(out=real["out"], in_=real["in_"], **kw)
        self.val[tid] += 16
        ins.then_inc(self.sem[tid], 16)
        self._commit(tid, self.val[tid], reads, writes)
        self.ninst += 1
        return ins

    def barrier(self):
        for e in self.eng:
            for tl, v in self.val.items():
                if v > 0:
                    if e == "sp" and False:
                        continue
                    if self.seen[e].get(tl, 0) < v:
                        self.eng[e].wait_ge(self.sem[tl], v)
                        self.seen[e][tl] = v
                        self.ninst += 1
        self.res.clear()

    def pe(self, name, **kw):
        return self.op("pe", name, **kw)

    def act(self, name, **kw):
        return self.op("act", name, **kw)

    def dve(self, name, **kw):
        return self.op("dve", name, **kw)

    def pool(self, name, **kw):
        return self.op("pool", name, **kw)


class Ring:
    def __init__(self, items):
        self.items = items
        self.i = 0

    def next(self):
        x = self.items[self.i % len(self.items)]
        self.i += 1
        return x


class Scope:
    def __init__(self, nc):
        self.nc = nc
        self.stack = []
        self.n = 0

    def sb(self, shape, dt=F32, name=None):
        self.n += 1
        g = self.nc.sbuf_tensor("%s_%d" % (name or "sb", self.n), list(shape), dt)
        t = g.__enter__()
        self.stack.append(g)
        return t

    def ps(self, shape, dt=F32, name=None):
        self.n += 1
        g = self.nc.psum_tensor("%s_%d" % (name or "ps", self.n), list(shape), dt)
        t = g.__enter__()
        PSUM_KEYS.add(t.name)
        self.stack.append(g)
        return t

    def sbring(self, n, shape, dt=F32, name=None):
        return Ring([self.sb(shape, dt, name) for _ in range(n)])

    def psring(self, n, shape, dt=F32, name=None):
        return Ring([self.ps(shape, dt, name) for _ in range(n)])

    def mark(self):
        return len(self.stack)

    def release(self, mark):
        while len(self.stack) > mark:
            self.stack.pop().__exit__(None, None, None)


class MK:
    def __init__(self, layers=(0, 1, 2, 3), phases="all", debug=()):
        self.layers = list(layers)
        self.phases = phases
        self.debug = set(debug)
        nc = bass.Bass("TRN2", target_bir_lowering=False)
        self.nc = nc
        self.s = Sched(nc)
        self.sc = Scope(nc)
        di = lambda n, sh, dt=F32: nc.dram_tensor(n, list(sh), dt, kind="ExternalInput").ap()
        dscr = lambda n, sh, dt=F32: nc.dram_tensor(n, list(sh), dt, kind="Internal").ap()
        self._in_shapes = dict(
            x=[SEQ, D], meta_tokens=[NMETA, D], norm1_w=[DEPTH, D], w_in=[DEPTH, D, INW], q_norm_w=[DEPTH, 64],
            k_norm_w=[DEPTH, 64], attn_norm_w=[DEPTH, 512], hgrn_lb=[2, DEPTH, 512], hgrn_norm_w=[DEPTH, 512],
            conv_w=[DEPTH, 768, 7], conv_b=[DEPTH, 768], dt_bias=[DEPTH, 16], a_log=[DEPTH, 16], d_skip=[DEPTH, 8],
            ssm_norm_w=[DEPTH, 512], w_out=[DEPTH, 1536, D], norm2_w=[DEPTH, D], router_w=[DEPTH, D, NEXP],
            w_gate=[MOE_DEPTH, NEXP, D, FF], w_up=[MOE_DEPTH, NEXP, D, FF], w_down=[MOE_DEPTH, NEXP, FF, D],
            c_ident=[128, 128], c_cs=[L, 64], c_masks=[128, 8, 128])
        self._in = {}
        self.y = nc.dram_tensor("y", [L, D], F32, kind="ExternalOutput").ap()
        self.qT_d = dscr("qT_d", [128, 4, L], BF16)
        self.kT_d = dscr("kT_d", [128, L], BF16)
        self.V_d = dscr("V_d", [L, 130], BF16)
        self.mix_d = dscr("mix_d", [L, 1536])
        self.hn_d = dscr("hn_d", [L, D])
        self.zs_d = dscr("zs_d", [L, 512])
        self.hq_d = dscr("hq_d", [L, 512])
        self.hgk_d = dscr("hgk_d", [2, 2, L, 512])
        self.hv_d = dscr("hv_d", [L, 512])
        self.hgate_d = dscr("hgate_d", [L, 512])
        self.yh_d = dscr("yh_d", [L, 512])
        self.dtg_d = dscr("dtg_d", [L, 32])
        self.xbcT_d = dscr("xbcT_d", [128, 6, L + 6])
        self.xcT_d = dscr("xcT_d", [128, 6, L])
        self.ys_d = dscr("ys_d", [L, 512])
        self.gate_d = dscr("gate_d", [16, 520])
        self.idx_d = dscr("idx_d", [16, 520], I32)
        self.sc_persist = self.sc
        self.dbg = {}

    def __getattr__(self, name):
        alias = {"meta": "meta_tokens", "ident_d": "c_ident", "cs_d": "c_cs", "masks_d": "c_masks"}
        n = alias.get(name, name)
        shapes = self.__dict__.get("_in_shapes", {})
        if n in shapes:
            if n not in self._in:
                self._in[n] = self.nc.dram_tensor(n, list(shapes[n]), F32, kind="ExternalInput").ap()
            return self._in[n]
        raise AttributeError(name)

    def h_src(self, l, t):
        R, o = tile_rows(t), tile_off(t)
        if l == 0 and self.layers[0] == 0:
            return self.meta[0:R, :] if t == 0 else self.x[o - NMETA:o - NMETA + R, :]
        return self.y[o:o + R, :]

    def hkey(self, t):
        return ("h", t)

    def load_consts(self):
        s, sc = self.s, self.sc
        self.ident = sc.sb([128, 128], name="ident")
        s.dma("sp", self.ident[:, :], self.ident_d[:, :])
        self.masks = sc.sb([128, 8, 128], name="masks")
        s.dma("sp", self.masks[:, :, :], self.masks_d[:, :, :])
        self.ones = sc.sb([128, 128], name="ones")
        self.affT = sc.sb([16, L], name="affT")
        s.pool("memset", ap=K(self.ones[:, :], self.ones.name), constant=1.0, _w=[self.ones.name])

    def rms_rstd(self, ssq, R, n, out=None):
        s = self.s
        s.dve("tensor_scalar", out=ssq, in0=ssq, scalar1=1.0 / n, scalar2=EPS, op0=ALU.mult, op1=ALU.add)
        s.act("activation", out=ssq, in_=ssq, func=AF.Ln)
        s.act("activation", out=ssq, in_=ssq, func=AF.Exp, scale=-0.5)

    def norm_transpose(self, l, li, t, rings, wbc):
        s = self.s
        R = tile_rows(t)
        xt = rings["xt"].next()
        s.dma("sp", xt[:R, :], K(self.h_src(li, t), self.hkey(t)))
        junk = rings["junk"].next()
        ssq = rings["ssq"].next()
        s.act("activation", out=junk[:R, :], in_=xt[:R, :], func=AF.Square, accum_out=ssq[:R, 0:1])
        self.rms_rstd(ssq[:R, 0:1], R, D)
        u = rings["u"].next()
        s.dve("scalar_tensor_tensor", out=u[:R, :], in0=xt[:R, :], scalar=ssq[:R, 0:1], in1=wbc[:R, :],
              op0=ALU.mult, op1=ALU.mult)
        psT = rings["psT"].next()
        for k in range(8):
            s.pe("transpose", out=psT[:, k * 128:k * 128 + R], in_=u[:R, k * 128:(k + 1) * 128],
                 identity=self.ident[:R, :R])
        uT = rings["uT"].next()
        pv = psT[:, :].rearrange("p (k r) -> p k r", k=8)
        s.act("copy", out=uT[:, :, :R], in_=pv[:, :, :R])
        return uT

    def mk_norm_rings(self, sc):
        return dict(
            xt=sc.sbring(2, [128, D], name="xt"), junk=sc.sbring(1, [128, D], name="junk"),
            ssq=sc.sbring(2, [128, 1], name="ssq"), u=sc.sbring(2, [128, D], name="u"),
            psT=sc.psring(1, [128, 1024], name="psT"), uT=sc.sbring(2, [128, 8, 128], BF16, name="uT"))

    def phase_A1(self, l, li):
        s, sc = self.s, self.sc
        m = sc.mark()
        wA = sc.sb([128, 8, 768], BF16, name="wA")
        s.dma("pool", wA[:, :, :], self.w_in[l, :, 0:768].rearrange("(k p) n -> p k n", p=128))
        w1bc = sc.sb([128, D], name="w1bc")
        s.dma("sp", w1bc[:, :], self.norm1_w[l:l + 1, :].to_broadcast([128, D]))
        qkw = sc.sb([128, 10, 64], name="qkw")
        s.dma("sp", K(qkw[:, 0:8, :], qkw.name), self.q_norm_w[l:l + 1, :].unsqueeze(1).to_broadcast([128, 8, 64]))
        s.dma("sp", K(qkw[:, 8:10, :], qkw.name), self.k_norm_w[l:l + 1, :].unsqueeze(1).to_broadcast([128, 2, 64]))
        rings = self.mk_norm_rings(sc)
        psQ = sc.psring(2, [128, 512], name="psQ")
        psKV = sc.psring(2, [128, 512], name="psKV")
        qk_r = sc.sbring(2, [128, 10, 64], name="qk")
        sq_r = sc.sbring(1, [128, 10, 64], name="sq")
        hs_r = sc.sbring(2, [128, 10], name="hs")
        vt_r = sc.sbring(2, [128, 2, 65], BF16, name="vt")
        cs_r = sc.sbring(2, [128, 64], name="cs")
        tmp_r = sc.sbring(4, [128, 8, 32], name="rt")
        qrp_r = sc.sbring(2, [128, 5, 128], name="qrp")
        psT2 = sc.psring(1, [128, 5 * 128], name="psT2")
        qkT_r = sc.sbring(2, [128, 5, 128], BF16, name="qkT")
        import os
        cut = int(os.environ.get("MK_CUT", "99"))
        for t in range(int(os.environ.get("MK_NT", NT))):
            R, o = tile_rows(t), tile_off(t)
            uT = self.norm_transpose(l, li, t, rings, w1bc)
            if cut <= 1:
                continue
            pq = psQ.next()
            pkv = psKV.next()
            for k in range(8):
                s.pe("matmul", out=pq[:R, :], lhsT=uT[:, k, :R], rhs=wA[:, k, 0:512], start=(k == 0), stop=(k == 7))
            for k in range(8):
                s.pe("matmul", out=pkv[:R, 0:256], lhsT=uT[:, k, :R], rhs=wA[:, k, 512:768], start=(k == 0), stop=(k == 7))
            qk = qk_r.next()
            qkf = qk[:, :, :].rearrange("p h d -> p (h d)")
            s.act("copy", out=K(qkf[:R, 0:512], qk.name), in_=pq[:R, :])
            s.act("copy", out=K(qkf[:R, 512:640], qk.name), in_=pkv[:R, 0:128])
            if cut <= 2:
                continue
            vt = vt_r.next()
            s.dve("tensor_copy", out=K(vt[:R, :, 0:64], vt.name), in_=pkv[:R, 128:256].rearrange("p (g d) -> p g d", g=2))
            s.pool("memset", ap=K(vt[:R, :, 64:65], vt.name), constant=1.0, _w=[vt.name])
            s.dma(STQ, K(self.V_d[o:o + R, :], ("V", t)), vt[:R, :, :].rearrange("p g d -> p (g d)"))
            if cut <= 3:
                continue
            sq = sq_r.next()
            s.pool("tensor_tensor", out=sq[:R, :, :], in0=qk[:R, :, :], in1=qk[:R, :, :], op=ALU.mult)
            hs = hs_r.next()
            s.dve("tensor_reduce", out=hs[:R, :], in_=sq[:R, :, :], axis=AX.X, op=ALU.add)
            self.rms_rstd(hs[:R, :], R, 64)
            s.dve("tensor_tensor", out=qk[:R, :, :], in0=qk[:R, :, :], in1=hs[:R, :].unsqueeze(2).to_broadcast([R, 10, 64]), op=ALU.mult)
            s.pool("tensor_tensor", out=qk[:R, :, :], in0=qk[:R, :, :], in1=qkw[:R, :, :], op=ALU.mult)
            if cut <= 4:
                continue
            cs = cs_r.next()
            s.dma("sp", cs[:R, :], self.cs_d[o:o + R, :])
            qrp = qrp_r.next()
            for (h0, nh, oview) in ((0, 8, qrp[:R, 0:4, :].rearrange("p s (g d) -> p g s d", g=2)),
                                   (8, 2, qrp[:R, 4:5, :].rearrange("p s (g d) -> p g s d", g=2))):
                if nh == 8:
                    xin = qk[:R, 0:8, :].rearrange("p (g r) d -> p g r d", g=2)
                    nr = 4
                else:
                    xin = qk[:R, 8:10, :].rearrange("p (g r) d -> p g r d", g=2)
                    nr = 1
                cb = cs[:R, 0:32].unsqueeze(1).unsqueeze(1).to_broadcast([R, 2, nr, 32])
                sb_ = cs[:R, 32:64].unsqueeze(1).unsqueeze(1).to_broadcast([R, 2, nr, 32])
                x1, x2 = xin[:, :, :, 0:32], xin[:, :, :, 32:64]
                t1, t2, t3, t4 = (tmp_r.next() for _ in range(4))
                v = lambda tt: tt[:R, 0:2 * nr, :].rearrange("p (g r) d -> p g r d", g=2)
                s.dve("tensor_tensor", out=K(v(t1), t1.name), in0=K(x1, qk.name), in1=K(cb, cs.name), op=ALU.mult)
                s.pool("tensor_tensor", out=K(v(t2), t2.name), in0=K(x2, qk.name), in1=K(sb_, cs.name), op=ALU.mult)
                s.dve("tensor_tensor", out=K(oview[:, :, :, 0:32], qrp.name), in0=K(v(t1), t1.name), in1=K(v(t2), t2.name), op=ALU.subtract)
                s.pool("tensor_tensor", out=K(v(t3), t3.name), in0=K(x1, qk.name), in1=K(sb_, cs.name), op=ALU.mult)
                s.dve("tensor_tensor", out=K(v(t4), t4.name), in0=K(x2, qk.name), in1=K(cb, cs.name), op=ALU.mult)
                s.pool("tensor_tensor", out=K(oview[:, :, :, 32:64], qrp.name), in0=K(v(t3), t3.name), in1=K(v(t4), t4.name), op=ALU.add)
            if cut <= 5:
                continue
            pT = psT2.next()
            for j in range(5):
                s.pe("transpose", out=pT[:, j * 128:j * 128 + R], in_=qrp[:R, j, :], identity=self.ident[:R, :R])
            qkT = qkT_r.next()
            s.act("copy", out=qkT[:, :, :R], in_=pT[:, :].rearrange("p (j r) -> p j r", j=5)[:, :, :R])
            s.dma(STQ, K(self.qT_d[:, :, o:o + R], ("qT", t)), qkT[:, 0:4, :R])
            s.dma(STQ, K(self.kT_d[:, o:o + R], ("kT", t)), qkT[:, 4, :R])
        s.barrier()
        sc.release(m)

    def phase_B1(self, l, li):
        s, sc = self.s, self.sc
        m = sc.mark()
        kT = sc.sb([128, L], BF16, name="kT")
        s.dma("sp", kT[:, :], K(self.kT_d[:, :], "kT_all"), _r=[("kT", t) for t in range(NT)])
        V = sc.sb([128, NT, 130], BF16, name="V")
        for q4 in range(4):
            s.dma("sp", K(V[:, 8 * q4:8 * q4 + 8, :], V.name),
                  K(self.V_d[NMETA + 1024 * q4:NMETA + 1024 * (q4 + 1), :].rearrange("(t p) c -> p t c", p=128), "V_all"),
                  _r=[("V", t) for t in range(NT)])
        s.dma("sp", K(V[0:NMETA, 32, :], V.name), K(self.V_d[0:NMETA, :], "V_all"))
        anw = sc.sb([128, 512], name="anw")
        s.dma("sp", anw[:, :], self.attn_norm_w[l:l + 1, :].to_broadcast([128, 512]))
        qT_r = sc.sbring(2, [128, 4, 512], BF16, name="qTb")
        psS = sc.psring(2, [128, 1024], name="psS")
        pT_r = sc.sbring(3, [128, 1024], BF16, name="pT")
        psO = sc.psring(2, [128, 512], name="psO")
        oT_r = sc.sbring(2, [128, 512], name="oT")
        psB = sc.psring(2, [128, 512], name="psB")
        att_r = sc.sbring(2, [128, 4, 512], name="att")
        rc_r = sc.sbring(2, [128, 1], name="rc")
        junk = sc.sb([128, 512], name="junkb")
        ssq_r = sc.sbring(2, [128, 1], name="ssqb")
        qblocks = [(0, NMETA)] + [(NMETA + 512 * b, 512) for b in range(8)]
        ksteps = [[(2 * i, NMETA + 256 * i, 128), (2 * i + 1, NMETA + 256 * i + 128, 128)] for i in range(16)] + [[(32, 0, NMETA)]]
        for (qo, NQ) in qblocks:
            qT = qT_r.next()
            s.dma("sp", qT[:, :, :NQ], K(self.qT_d[:, :, qo:qo + NQ], "qT_all"), _r=[("qT", t) for t in range(NT)])
            att = att_r.next()
            nsub = (NQ + 127) // 128
            for g in range(2):
                for r in range(4):
                    h = g * 4 + r
                    po = psO.next()
                    for si, step in enumerate(ksteps):
                        ps = psS.next()
                        for j, (kt, ko, KR) in enumerate(step):
                            s.pe("matmul", out=ps[:KR, j * 512:j * 512 + NQ], lhsT=kT[g * 64:(g + 1) * 64, ko:ko + KR],
                                 rhs=qT[g * 64:(g + 1) * 64, r, :NQ], start=True, stop=True)
                        pT = pT_r.next()
                        KR = step[0][2]
                        if len(step) == 2 and NQ == 512:
                            s.act("activation", out=pT[:KR, :], in_=ps[:KR, :], func=AF.Exp, scale=0.125)
                        else:
                            for j in range(len(step)):
                                s.act("activation", out=pT[:KR, j * 512:j * 512 + NQ], in_=ps[:KR, j * 512:j * 512 + NQ],
                                      func=AF.Exp, scale=0.125)
                        for j, (kt, ko, KR) in enumerate(step):
                            s.pe("matmul", out=po[:65, :NQ], lhsT=V[:KR, kt, g * 65:(g + 1) * 65], rhs=pT[:KR, j * 512:j * 512 + NQ],
                                 start=(si == 0 and j == 0), stop=(si == len(ksteps) - 1))
                    oT = oT_r.next()
                    s.dve("tensor_copy", out=oT[:65, :NQ], in_=po[:65, :NQ])
                    pb = psB.next()
                    for sb_ in range(nsub):
                        n = min(128, NQ - sb_ * 128)
                        s.pe("transpose", out=pb[:n, sb_ * 65:sb_ * 65 + 65], in_=oT[:65, sb_ * 128:sb_ * 128 + n],
                             identity=self.ident[:65, :65])
                    for sb_ in range(nsub):
                        n = min(128, NQ - sb_ * 128)
                        rc = rc_r.next()
                        s.dve("reciprocal", out=rc[:n, :], in_=pb[:n, sb_ * 65 + 64:sb_ * 65 + 65])
                        s.dve("tensor_scalar", out=K(att[:n, sb_, h * 64:(h + 1) * 64], att.name),
                              in0=pb[:n, sb_ * 65:sb_ * 65 + 64], scalar1=rc[:n, 0:1], scalar2=None, op0=ALU.mult)
            for sb_ in range(nsub):
                n = min(128, NQ - sb_ * 128)
                ssq = ssq_r.next()
                s.act("activation", out=junk[:n, :], in_=K(att[:n, sb_, :], att.name), func=AF.Square, accum_out=ssq[:n, 0:1])
                self.rms_rstd(ssq[:n, 0:1], n, 512)
                s.dve("scalar_tensor_tensor", out=K(att[:n, sb_, :], att.name), in0=K(att[:n, sb_, :], att.name),
                      scalar=ssq[:n, 0:1], in1=anw[:n, :], op0=ALU.mult, op1=ALU.mult)
                o = qo + sb_ * 128
                s.dma(STQ, K(self.mix_d[o:o + n, 0:512], ("mixA", o)), K(att[:n, sb_, :], att.name))
        s.barrier()
        sc.release(m)

    def phase_A3(self, l, li):
        s, sc = self.s, self.sc
        m = sc.mark()
        wS = sc.sb([128, 8, 1296], BF16, name="wS")
        s.dma("pool", wS[:, :, :], self.w_in[l, :, 3328:4624].rearrange("(k p) n -> p k n", p=128))
        w1bc = sc.sb([128, D], name="w1bc")
        s.dma("sp", w1bc[:, :], self.norm1_w[l:l + 1, :].to_broadcast([128, D]))
        dtb = sc.sb([128, 16], name="dtb")
        s.dma("sp", dtb[:, :], self.dt_bias[l:l + 1, :].to_broadcast([128, 16]))
        Abc = sc.sb([128, 16], name="Abc")
        s.dma("sp", Abc[:, :], self.a_log[l:l + 1, :].to_broadcast([128, 16]))
        s.act("activation", out=Abc[:, :], in_=Abc[:, :], func=AF.Exp)
        s.dve("tensor_scalar", out=Abc[:, :], in0=Abc[:, :], scalar1=-1.0, scalar2=None, op0=ALU.mult)
        zero = sc.sb([128, 6, 3], name="zero")
        s.dve("memset", ap=zero[:, :, :], constant=0.0, _w=[zero.name])
        s.dma(STQ, K(self.xbcT_d[:, :, 0:3], ("xbcT", "h0")), zero[:, :, :])
        s.dma(STQ, K(self.xbcT_d[:, :, 3 + L:6 + L], ("xbcT", "h1")), zero[:, :, :])
        rings = self.mk_norm_rings(sc)
        pz_r = sc.psring(2, [128, 512], name="pz")
        pd_r = sc.psring(1, [128, 512], name="pd")
        px_r = sc.psring(1, [128, 1024], name="px")
        zs_r = sc.sbring(2, [128, 512], name="zs")
        dtg_r = sc.sbring(2, [128, 32], name="dtg")
        xT_r = sc.sbring(2, [128, 6, 128], name="xTs")
        for t in range(NT):
            R, o = tile_rows(t), tile_off(t)
            uT = self.norm_transpose(l, li, t, rings, w1bc)
            pz = pz_r.next()
            for k in range(8):
                s.pe("matmul", out=pz[:R, :], lhsT=uT[:, k, :R], rhs=wS[:, k, 0:512], start=(k == 0), stop=(k == 7))
            zs = zs_r.next()
            s.act("activation", out=zs[:R, :], in_=pz[:R, :], func=AF.Silu)
            s.dma(STQ, K(self.zs_d[o:o + R, :], ("zs", t)), zs[:R, :])
            pd = pd_r.next()
            for k in range(8):
                s.pe("matmul", out=pd[:R, 0:16], lhsT=uT[:, k, :R], rhs=wS[:, k, 1280:1296], start=(k == 0), stop=(k == 7))
            dtg = dtg_r.next()
            s.dve("tensor_tensor", out=K(dtg[:R, 0:16], dtg.name), in0=pd[:R, 0:16], in1=dtb[:R, :], op=ALU.add)
            s.act("activation", out=K(dtg[:R, 0:16], dtg.name), in_=K(dtg[:R, 0:16], dtg.name), func=AF.Exp)
            s.act("activation", out=K(dtg[:R, 0:16], dtg.name), in_=K(dtg[:R, 0:16], dtg.name), func=AF.Ln, bias=1.0)
            s.dve("tensor_tensor", out=K(dtg[:R, 16:32], dtg.name), in0=K(dtg[:R, 0:16], dtg.name), in1=Abc[:R, :], op=ALU.mult)
            s.dma(STQ, K(self.dtg_d[o:o + R, :], ("dtg", t)), dtg[:R, :])
            px = px_r.next()
            for cc in range(6):
                for k in range(8):
                    s.pe("matmul", out=px[:, cc * 128:cc * 128 + R], lhsT=wS[:, k, 512 + cc * 128:512 + (cc + 1) * 128],
                         rhs=uT[:, k, :R], start=(k == 0), stop=(k == 7))
            xT = xT_r.next()
            s.act("copy", out=xT[:, :, :R], in_=px[:, 0:768].rearrange("p (c r) -> p c r", c=6)[:, :, :R])
            s.dma(STQ, K(self.xbcT_d[:, :, 3 + o:3 + o + R], ("xbcT", t)), xT[:, :, :R])
        s.barrier()
        sc.release(m)

    def phase_B3a(self, l, li):
        s, sc = self.s, self.sc
        m = sc.mark()
        cw = sc.sb([128, 6, 7], name="cw")
        s.dma("sp", cw[:, :, :], self.conv_w[l, :, :].rearrange("(c p) k -> p c k", p=128))
        cb = sc.sb([128, 6], name="cb")
        s.dma("sp", cb[:, :], self.conv_b[l, :].rearrange("(c p) -> p c", p=128), allow_slow_non_contiguous=True)
        xin_r = sc.sbring(2, [128, L + 6], name="xin")
        acc_r = sc.sbring(2, [128, L], name="acc")
        for cc in range(6):
            xin = xin_r.next()
            s.dma("sp", xin[:, :], K(self.xbcT_d[:, cc, :], "xbcT_all"))
            acc = acc_r.next()
            eng = "dve"
            s.op(eng, "tensor_scalar", out=acc[:, :], in0=xin[:, 0:L], scalar1=K(cw[:, cc, 0:1], cw.name),
                 scalar2=K(cb[:, cc:cc + 1], cb.name), op0=ALU.mult, op1=ALU.add)
            for k in range(1, 7):
                s.op(eng, "scalar_tensor_tensor", out=acc[:, :], in0=xin[:, k:k + L], scalar=K(cw[:, cc, k:k + 1], cw.name),
                     in1=acc[:, :], op0=ALU.mult, op1=ALU.add)
            s.act("activation", out=acc[:, :], in_=acc[:, :], func=AF.Silu)
            s.dma(STQ, K(self.xcT_d[:, cc, :], ("xcT", cc)), acc[:, :])
        s.barrier()
        sc.release(m)

    def phase_B3b(self, l, li):
        s, sc = self.s, self.sc
        m = sc.mark()
        dsk = sc.sb([128, 8], name="dsk")
        s.dma("sp", dsk[:, :], self.d_skip[l:l + 1, :].to_broadcast([128, 8]))
        snw = sc.sb([128, 512], name="snw")
        s.dma("sp", snw[:, :], self.ssm_norm_w[l:l + 1, :].to_broadcast([128, 512]))
        S = sc.sb([128, 4, 64], name="Sst")
        xc_r = sc.sbring(2, [128, 6, 128], name="xc")
        dtg_r = sc.sbring(2, [128, 32], name="dtgl")
        pXt_r = sc.psring(1, [128, 1024], name="pXt")
        xtok_r = sc.sbring(2, [128, 640], name="xtok")
        xdt_r = sc.sbring(2, [128, 8, 64], name="xdt")
        pGS_r = sc.psring(1, [128, 512], name="pGS")
        cum_r = sc.sbring(2, [128, 8], name="cum")
        sm_r = sc.sbring(2, [128, 5, 8], name="smv")
        ST_r = sc.sbring(2, [128, 2, 128], name="ST")
        gbc_r = sc.sbring(2, [128, 8, 128], name="gbc")
        pDm_r = sc.psring(2, [128, 512], name="pDm")
        attd_r = sc.sbring(3, [128, 128], name="attd")
        attm_r = sc.sbring(3, [128, 128], name="attm")
        py_r = sc.psring(1, [128, 512], name="py")
        pI_r = sc.psring(1, [128, 512], name="pI")
        pSt_r = sc.psring(1, [128, 512], name="pSt")
        yt_r = sc.sbring(2, [128, 512], name="yt")
        yf_r = sc.sbring(2, [128, 512], name="yfl")
        zs_r = sc.sbring(2, [128, 512], name="zsl")
        xdw_r = sc.sbring(2, [128, 8, 64], name="xdw")
        tmp_r = sc.sbring(2, [128, 4, 64], name="tmps")
        junk = sc.sb([128, 512], name="junks")
        ssq_r = sc.sbring(2, [128, 1], name="ssqs")
        cut3 = int(os.environ.get("MK_B3CUT", "99"))
        nd = int(os.environ.get("MK_B3ND", "2"))
        for d in range(nd):
            s.dve("memset", ap=S[:, :, :], constant=0.0, _w=[S.name])
            order = range(NT) if d == 0 else range(NT - 1, -1, -1)
            order = list(order)[:int(os.environ.get("MK_NT", NT))]
            U = self.masks[:, d, :]
            NEG = self.masks[:, 2 + d, :]
            for t in order:
                R, o = tile_rows(t), tile_off(t)
                xc = xc_r.next()
                s.dma("sp", xc[:, :, :R], K(self.xcT_d[:, :, o:o + R], "xcT_all"))
                dtg = dtg_r.next()
                s.dma("sp", dtg[:R, :], K(self.dtg_d[o:o + R, :], "dtg_all"))
                pXt = pXt_r.next()
                for cc in range(5):
                    s.pe("transpose", out=pXt[:R, cc * 128:(cc + 1) * 128], in_=xc[:, cc, :R], identity=self.ident[:, :])
                xtok = xtok_r.next()
                s.act("copy", out=xtok[:R, :], in_=pXt[:R, 0:640])
                xdt = xdt_r.next()
                xv = xtok[:R, 0:512].rearrange("p (h q) -> p h q", h=8)
                s.dve("tensor_tensor", out=xdt[:R, :, :], in0=K(xv, xtok.name),
                      in1=K(dtg[:R, d * 8:d * 8 + 8].unsqueeze(2).to_broadcast([R, 8, 64]), dtg.name), op=ALU.mult)
                g8 = dtg[:R, 16 + d * 8:16 + d * 8 + 8]
                if cut3 <= 1:
                    continue
                pGS = pGS_r.next()
                s.pe("matmul", out=pGS[:R, 0:8], lhsT=U[:R, :R], rhs=K(g8, dtg.name), start=True, stop=True)
                pTot = pGS[:, 384:392]
                s.pe("matmul", out=pTot[:, 0:8], lhsT=self.ones[:R, :], rhs=K(g8, dtg.name), start=True, stop=True)
                sm = sm_r.next()
                s.act("copy", out=K(sm[:R, 0, :], sm.name), in_=pGS[:R, 0:8])
                s.dve("tensor_scalar", out=K(sm[:R, 1, :], sm.name), in0=K(sm[:R, 0, :], sm.name), scalar1=-1.0, scalar2=None, op0=ALU.mult)
                s.dve("tensor_tensor", out=K(sm[:R, 2, :], sm.name), in0=pTot[:R, 0:8], in1=K(sm[:R, 0, :], sm.name), op=ALU.subtract)
                s.act("activation", out=K(sm[:R, 2, :], sm.name), in_=K(sm[:R, 2, :], sm.name), func=AF.Exp)
                s.act("activation", out=K(sm[:R, 3, :], sm.name), in_=K(sm[:R, 0, :], sm.name), func=AF.Exp)
                s.act("activation", out=K(sm[:, 4, :], sm.name), in_=pTot[:, 0:8], func=AF.Exp)
                if cut3 <= 2:
                    continue
                pI = pI_r.next()
                stp = (pGS[:R, 128:128 + R], pI[:R, 0:R])
                for g in range(2):
                    s.pe("matmul", out=stp[g], lhsT=xc[g * 64:(g + 1) * 64, 4, :R],
                         rhs=xc[g * 64:(g + 1) * 64, 5, :R], start=True, stop=True)
                if cut3 == 3 and os.environ.get("MK_B3SUB") == "1":
                    continue
                ST = ST_r.next()
                for g in range(2):
                    s.dve("tensor_tensor", out=K(ST[:R, g, :R], ST.name), in0=stp[g], in1=K(U[:R, :R], self.masks.name), op=ALU.mult)
                if cut3 == 3 and os.environ.get("MK_B3SUB") == "2":
                    continue
                gbc = gbc_r.next()
                s.dve("tensor_tensor", out=gbc[:R, :, :R], in0=K(self.ones[:R, :R].unsqueeze(1).to_broadcast([R, 8, R]), self.ones.name),
                       in1=K(g8.unsqueeze(2).to_broadcast([R, 8, R]), dtg.name), op=ALU.mult)
                if cut3 <= 3:
                    continue
                py = py_r.next()
                for h in range(8):
                    g = h // 4
                    pDm = pDm_r.next()
                    s.pe("matmul", out=pDm[:R, :R], lhsT=gbc[:R, h, :R], rhs=K(U[:R, :R], self.masks.name), start=True, stop=False)
                    s.pe("matmul", out=pDm[:R, :R], lhsT=self.ident[:R, :R], rhs=K(NEG[:R, :R], self.masks.name), start=False, stop=True)
                    attd = attd_r.next()
                    s.act("activation", out=attd[:R, :R], in_=pDm[:R, :R], func=AF.Exp, bias=K(sm[:R, 1, h:h + 1], sm.name))
                    attm = attm_r.next()
                    s.pool("tensor_tensor", out=attm[:R, :R], in0=attd[:R, :R], in1=K(ST[:R, g, :R], ST.name), op=ALU.mult)
                    s.pe("matmul", out=py[:R, h * 64:(h + 1) * 64], lhsT=attm[:R, :R], rhs=K(xdt[:R, h, :], xdt.name), start=True, stop=True)
                if cut3 <= 4:
                    continue
                ip = (pI[:R, 0:256], pGS[:R, 0:256])
                for g in range(2):
                    s.pe("matmul", out=ip[g], lhsT=xc[g * 64:(g + 1) * 64, 5, :R],
                         rhs=S[g * 64:(g + 1) * 64, :, :].rearrange("p r q -> p (r q)"), start=True, stop=True)
                yt = yt_r.next()
                for g in range(2):
                    s.dve("tensor_tensor", out=K(yt[:R, g * 256:(g + 1) * 256].rearrange("p (h q) -> p h q", h=4), yt.name),
                          in0=ip[g].rearrange("p (h q) -> p h q", h=4),
                          in1=K(sm[:R, 3, g * 4:g * 4 + 4].unsqueeze(2).to_broadcast([R, 4, 64]), sm.name), op=ALU.mult)
                s.dve("tensor_tensor", out=yt[:R, :], in0=yt[:R, :], in1=py[:R, :], op=ALU.add)
                if cut3 <= 5:
                    continue
                xdw = xdw_r.next()
                s.pool("tensor_tensor", out=xdw[:R, :, :], in0=xdt[:R, :, :],
                       in1=K(sm[:R, 2, :].unsqueeze(2).to_broadcast([R, 8, 64]), sm.name), op=ALU.mult)
                pSt = pSt_r.next()
                s.pe("matmul", out=pSt[:, :], lhsT=xtok[:R, 512:640], rhs=xdw[:R, :, :].rearrange("p h q -> p (h q)"), start=True, stop=True)
                for g in range(2):
                    gs = slice(g * 64, (g + 1) * 64)
                    tmp = tmp_r.next()
                    s.dve("tensor_tensor", out=K(tmp[gs, :, :], tmp.name), in0=K(S[gs, :, :], S.name),
                          in1=K(sm[gs, 4, g * 4:g * 4 + 4].unsqueeze(2).to_broadcast([64, 4, 64]), sm.name), op=ALU.mult)
                    s.dve("tensor_tensor", out=K(S[gs, :, :], S.name), in0=K(tmp[gs, :, :], tmp.name),
                          in1=pSt[gs, g * 256:(g + 1) * 256].rearrange("p (r q) -> p r q", r=4), op=ALU.add)
                if cut3 <= 6:
                    continue
                if d == 0:
                    s.dma(STQ, K(self.ys_d[o:o + R, :], ("ys", t)), yt[:R, :])
                else:
                    yf = yf_r.next()
                    s.dma("sp", yf[:R, :], K(self.ys_d[o:o + R, :], ("ys", t)))
                    zs = zs_r.next()
                    s.dma("sp", zs[:R, :], K(self.zs_d[o:o + R, :], "zs_all"))
                    s.dve("tensor_tensor", out=yt[:R, :], in0=yt[:R, :], in1=yf[:R, :], op=ALU.add)
                    s.pool("tensor_tensor", out=yf[:R, :].rearrange("p (h q) -> p h q", h=8), in0=K(xv, xtok.name),
                           in1=K(dsk[:R, :].unsqueeze(2).to_broadcast([R, 8, 64]), dsk.name), op=ALU.mult)
                    s.dve("tensor_tensor", out=yt[:R, :], in0=yt[:R, :], in1=yf[:R, :], op=ALU.add)
                    s.dve("tensor_tensor", out=yt[:R, :], in0=yt[:R, :], in1=zs[:R, :], op=ALU.mult)
                    ssq = ssq_r.next()
                    s.act("activation", out=junk[:R, :], in_=yt[:R, :], func=AF.Square, accum_out=ssq[:R, 0:1])
                    self.rms_rstd(ssq[:R, 0:1], R, 512)
                    s.dve("scalar_tensor_tensor", out=yt[:R, :], in0=yt[:R, :], scalar=ssq[:R, 0:1], in1=snw[:R, :],
                          op0=ALU.mult, op1=ALU.mult)
                    s.dma(STQ, K(self.mix_d[o:o + R, 1024:1536], ("mixC", t)), yt[:R, :])
            s.barrier()
        sc.release(m)

    def phase_A2(self, l, li):
        s, sc = self.s, self.sc
        m = sc.mark()
        wH = sc.sb([128, 8, 2560], BF16, name="wH")
        for k2 in range(4):
            s.dma("pool", K(wH[:, 2 * k2:2 * k2 + 2, :], wH.name),
                  self.w_in[l, 256 * k2:256 * (k2 + 1), 768:3328].rearrange("(k p) n -> p k n", p=128))
        w1bc = sc.sb([128, D], name="w1bc")
        s.dma("sp", w1bc[:, :], self.norm1_w[l:l + 1, :].to_broadcast([128, D]))
        lbe = sc.sb([128, 2, DEPTH, 512], name="lbe")
        for dd in range(2):
            s.dma("sp", K(lbe[:, dd, :, :], lbe.name), self.hgrn_lb[dd:dd + 1, :, :].to_broadcast([128, DEPTH, 512]))
        s.act("activation", out=lbe[:, :, :, :], in_=lbe[:, :, :, :], func=AF.Exp)
        lsum = sc.sb([128, 2, 512], name="lsum")
        lbb = sc.sb([128, 2, 512], name="lbb")
        s.dve("tensor_tensor", out=lsum[:, :, :], in0=K(lbe[:, :, 0, :], lbe.name), in1=K(lbe[:, :, 1, :], lbe.name), op=ALU.add)
        s.dve("tensor_tensor", out=lsum[:, :, :], in0=lsum[:, :, :], in1=K(lbe[:, :, 2, :], lbe.name), op=ALU.add)
        s.dve("tensor_tensor", out=lsum[:, :, :], in0=lsum[:, :, :], in1=K(lbe[:, :, 3, :], lbe.name), op=ALU.add)
        s.dve("reciprocal", out=lsum[:, :, :], in_=lsum[:, :, :])
        s.dve("memset", ap=lbb[:, :, :], constant=0.0, _w=[lbb.name])
        for lp in range(1, l + 1):
            s.dve("tensor_tensor", out=lbb[:, :, :], in0=lbb[:, :, :], in1=K(lbe[:, :, lp, :], lbe.name), op=ALU.add)
        s.dve("tensor_tensor", out=lbb[:, :, :], in0=lbb[:, :, :], in1=lsum[:, :, :], op=ALU.mult)
        s.dve("tensor_scalar", out=lbb[:, :, :], in0=lbb[:, :, :], scalar1=0.0, scalar2=1.0, op0=ALU.max, op1=ALU.min)
        rings = self.mk_norm_rings(sc)
        pp_r = sc.psring(3, [128, 512], name="pp")
        o_r = sc.sbring(3, [128, 512], name="oA2")
        e_r = sc.sbring(2, [128, 512], name="eA2")
        n_r = sc.sbring(2, [128, 512], name="nA2")
        f_r = sc.sbring(2, [128, 512], name="fA2")
        for t in range(NT):
            R, o = tile_rows(t), tile_off(t)
            uT = self.norm_transpose(l, li, t, rings, w1bc)

            def proj(ci):
                pp = pp_r.next()
                for k in range(8):
                    s.pe("matmul", out=pp[:R, :], lhsT=uT[:, k, :R], rhs=wH[:, k, ci * 512:(ci + 1) * 512],
                         start=(k == 0), stop=(k == 7))
                return pp
            pq = proj(0)
            ot = o_r.next()
            s.act("activation", out=ot[:R, :], in_=pq[:R, :], func=AF.Silu)
            s.dma(STQ, K(self.hq_d[o:o + R, :], ("hq", t)), ot[:R, :])
            for dd in range(2):
                pf = proj(1 + dd)
                e = e_r.next()
                s.dve("tensor_scalar", out=e[:R, :], in0=pf[:R, :], scalar1=-1.0, scalar2=30.0, op0=ALU.mult, op1=ALU.min)
                s.act("activation", out=e[:R, :], in_=e[:R, :], func=AF.Exp)
                num = n_r.next()
                s.dve("tensor_tensor", out=num[:R, :], in0=e[:R, :], in1=K(lbb[:R, dd, :], lbb.name), op=ALU.mult)
                s.pool("tensor_scalar", out=e[:R, :], in0=e[:R, :], scalar1=1.0, scalar2=None, op0=ALU.add)
                s.dve("reciprocal", out=e[:R, :], in_=e[:R, :])
                f = f_r.next()
                s.dve("scalar_tensor_tensor", out=f[:R, :], in0=num[:R, :], scalar=1.0, in1=e[:R, :], op0=ALU.add, op1=ALU.mult)
                s.pool("tensor_scalar", out=f[:R, :], in0=f[:R, :], scalar1=1.0, scalar2=None, op0=ALU.min)
                s.act("activation", out=num[:R, :], in_=f[:R, :], func=AF.Ln)
                s.dma(STQ, K(self.hgk_d[dd, 0, o:o + R, :], ("hg", dd, t)), num[:R, :])
                s.pool("tensor_scalar", out=f[:R, :], in0=f[:R, :], scalar1=-1.0, scalar2=1.0, op0=ALU.mult, op1=ALU.add)
                s.dma(STQ, K(self.hgk_d[dd, 1, o:o + R, :], ("hk", dd, t)), f[:R, :])
            pi = proj(3)
            ot = o_r.next()
            s.act("copy", out=ot[:R, :], in_=pi[:R, :])
            s.dma(STQ, K(self.hv_d[o:o + R, :], ("hv", t)), ot[:R, :])
            pg = proj(4)
            ot = o_r.next()
            s.act("activation", out=ot[:R, :], in_=pg[:R, :], func=AF.Silu)
            s.dma(STQ, K(self.hgate_d[o:o + R, :], ("hgate", t)), ot[:R, :])
        s.barrier()
        sc.release(m)

    def phase_B2(self, l, li):
        s, sc = self.s, self.sc
        m = sc.mark()
        hnw = sc.sb([128, 512], name="hnw")
        s.dma("sp", hnw[:, :], self.hgrn_norm_w[l:l + 1, :].to_broadcast([128, 512]))
        mv = sc.sb([128, 2, 2, 2], name="mv")
        s.dve("memset", ap=mv[:, :, :, :], constant=0.0, _w=[mv.name])
        s.dve("memset", ap=K(mv[0:64, 0, 0, 0:1], mv.name), constant=1.0, _w=[mv.name])
        s.dve("memset", ap=K(mv[64:128, 0, 0, 1:2], mv.name), constant=1.0, _w=[mv.name])
        s.dve("memset", ap=K(mv[64:128, 1, 0, 0:1], mv.name), constant=1.0, _w=[mv.name])
        s.dve("memset", ap=K(mv[0:64, 1, 0, 1:2], mv.name), constant=1.0, _w=[mv.name])
        s.dve("memset", ap=K(mv[:, :, 1, 0:1], mv.name), constant=1.0, _w=[mv.name])
        S = sc.sb([128, 4, 128], name="Sgla")
        Sp = sc.sb([128, 4, 128], name="Spgla")
        q_r = sc.sbring(2, [128, 512], name="qg")
        g_r = sc.sbring(2, [128, 512], name="gg")
        k_r = sc.sbring(2, [128, 512], name="kg")
        v_r = sc.sbring(2, [128, 512], name="vg")
        pE_r = sc.psring(1, [128, 512], name="pE")
        ex_r = sc.sbring(2, [128, 512], name="exg")
        qt_r = sc.sbring(2, [128, 512], name="qtl")
        kt_r = sc.sbring(2, [128, 512], name="ktl")
        pT_r = sc.psring(1, [128, 1024], name="pTg")
        qkT_r = sc.sbring(2, [128, 8, 128], name="qkTg")
        pA_r = sc.psring(2, [128, 512], name="pAg")
        attS_r = sc.sbring(2, [128, 128], name="attS")
        att16_r = sc.sbring(2, [16, 16], name="att16")
        py_r = sc.psring(1, [128, 512], name="pyg")
        pV_r = sc.psring(1, [128, 512], name="pVg")
        ev_r = sc.sbring(2, [128, 4, 3], name="evg")
        pS_r = sc.psring(1, [128, 512], name="pSg")
        o_r = sc.sbring(2, [128, 512], name="og")
        yf_r = sc.sbring(2, [128, 512], name="yfg")
        gt_r = sc.sbring(2, [128, 512], name="gtg")
        sq_r = sc.sbring(1, [128, 512], name="sqg")
        hs_r = sc.sbring(2, [128, 4], name="hsg")
        for d in range(2):
            s.dve("memset", ap=S[:, :, :], constant=0.0, _w=[S.name])
            for a_ in attS_r.items:
                s.dve("memset", ap=a_[:, :], constant=0.0, _w=[a_.name])
            order = range(NT) if d == 0 else range(NT - 1, -1, -1)
            for t in order:
                R, o = tile_rows(t), tile_off(t)
                big = (R == 128)
                W = self.masks[:, 4 + d, :] if big else self.masks[:, 6 + d, :]
                M = self.masks[:, d, :]
                q, g, k, v = q_r.next(), g_r.next(), k_r.next(), v_r.next()
                s.dma("sp", q[:R, :], K(self.hq_d[o:o + R, :], "hq_all"))
                s.dma("sp", g[:R, :], K(self.hgk_d[d, 0, o:o + R, :], "hg_all"))
                s.dma("sp", k[:R, :], K(self.hgk_d[d, 1, o:o + R, :], "hk_all"))
                s.dma("sp", v[:R, :], K(self.hv_d[o:o + R, :], "hv_all"))
                pE = pE_r.next()
                s.pe("matmul", out=pE[:R, :], lhsT=K(W[:R, :R], self.masks.name), rhs=g[:R, :], start=True, stop=True)
                ex1, ex2 = ex_r.next(), ex_r.next()
                s.act("activation", out=ex1[:R, :], in_=pE[:R, :], func=AF.Exp)
                s.act("activation", out=ex2[:R, :], in_=pE[:R, :], func=AF.Exp, scale=-1.0)
                qt, kt = qt_r.next(), kt_r.next()
                s.dve("tensor_tensor", out=qt[:R, :], in0=q[:R, :], in1=ex1[:R, :], op=ALU.mult)
                s.pool("tensor_tensor", out=kt[:R, :], in0=k[:R, :], in1=ex2[:R, :], op=ALU.mult)
                pT = pT_r.next()
                for h in range(4):
                    s.pe("transpose", out=pT[:, h * 128:h * 128 + R], in_=qt[:R, h * 128:(h + 1) * 128], identity=self.ident[:R, :R])
                    s.pe("transpose", out=pT[:, 512 + h * 128:512 + h * 128 + R], in_=kt[:R, h * 128:(h + 1) * 128], identity=self.ident[:R, :R])
                qkT = qkT_r.next()
                s.act("copy", out=qkT[:, :, :R], in_=pT[:, :].rearrange("p (j r) -> p j r", j=8)[:, :, :R])
                pV = pV_r.next()
                for h in range(4):
                    s.pe("matmul", out=pV[:, h * 2:h * 2 + 2], lhsT=g[:R, h * 128:(h + 1) * 128],
                         rhs=K(mv[:R, d, 0 if big else 1, :], mv.name), start=True, stop=True)
                ev = ev_r.next()
                s.act("activation", out=K(ev[:, :, 0:2], ev.name), in_=pV[:, 0:8].rearrange("p (h c) -> p h c", h=4), func=AF.Exp)
                s.dve("tensor_tensor", out=K(ev[:, :, 2:3], ev.name), in0=K(ev[:, :, 0:1], ev.name), in1=K(ev[:, :, 1:2], ev.name), op=ALU.mult)
                s.dve("tensor_tensor", out=Sp[:, :, :], in0=S[:, :, :], in1=K(ev[:, :, 0:1].to_broadcast([128, 4, 128]), ev.name), op=ALU.mult)
                py = py_r.next()
                for h in range(4):
                    pA = pA_r.next()
                    s.pe("matmul", out=pA[:R, :R], lhsT=qkT[:, 4 + h, :R], rhs=qkT[:, h, :R], start=True, stop=True)
                    if big:
                        attS = attS_r.next()
                        if d == 0:
                            s.dve("tensor_tensor", out=K(attS[0:64, :], attS.name), in0=pA[0:64, 0:128], in1=K(M[0:64, :], self.masks.name), op=ALU.mult)
                            s.dve("tensor_tensor", out=K(attS[64:128, 64:128], attS.name), in0=pA[64:128, 64:128], in1=K(M[64:128, 64:128], self.masks.name), op=ALU.mult)
                        else:
                            s.dve("tensor_tensor", out=K(attS[:, 0:64], attS.name), in0=pA[:, 0:64], in1=K(M[:, 0:64], self.masks.name), op=ALU.mult)
                            s.dve("tensor_tensor", out=K(attS[64:128, 64:128], attS.name), in0=pA[64:128, 64:128], in1=K(M[64:128, 64:128], self.masks.name), op=ALU.mult)
                        aT = attS[:, :]
                    else:
                        a16 = att16_r.next()
                        s.dve("tensor_tensor", out=a16[:, :], in0=pA[:R, :R], in1=K(M[:R, :R], self.masks.name), op=ALU.mult)
                        aT = a16[:, :]
                    s.pe("matmul", out=py[:R, h * 128:(h + 1) * 128], lhsT=aT, rhs=v[:R, h * 128:(h + 1) * 128], start=True, stop=False)
                    s.pe("matmul", out=py[:R, h * 128:(h + 1) * 128], lhsT=qkT[:, h, :R], rhs=K(Sp[:, h, :], Sp.name), start=False, stop=True)
                pS = pS_r.next()
                for h in range(4):
                    s.pe("matmul", out=pS[:, h * 128:(h + 1) * 128], lhsT=kt[:R, h * 128:(h + 1) * 128], rhs=v[:R, h * 128:(h + 1) * 128],
                         start=True, stop=True)
                s.dve("tensor_tensor", out=S[:, :, :], in0=S[:, :, :], in1=K(ev[:, :, 2:3].to_broadcast([128, 4, 128]), ev.name), op=ALU.mult)
                s.dve("tensor_tensor", out=Sp[:, :, :], in0=pS[:, :].rearrange("p (h c) -> p h c", h=4),
                      in1=K(ev[:, :, 1:2].to_broadcast([128, 4, 128]), ev.name), op=ALU.mult)
                s.dve("tensor_tensor", out=S[:, :, :], in0=S[:, :, :], in1=Sp[:, :, :], op=ALU.add)
                ot = o_r.next()
                if d == 0:
                    s.act("copy", out=ot[:R, :], in_=py[:R, :])
                    s.dma(STQ, K(self.yh_d[o:o + R, :], ("yh", t)), ot[:R, :])
                else:
                    yf = yf_r.next()
                    s.dma("sp", yf[:R, :], K(self.yh_d[o:o + R, :], ("yh", t)))
                    gt = gt_r.next()
                    s.dma("sp", gt[:R, :], K(self.hgate_d[o:o + R, :], "hgate_all"))
                    s.dve("tensor_tensor", out=ot[:R, :], in0=yf[:R, :], in1=py[:R, :], op=ALU.add)
                    sq = sq_r.next()
                    s.pool("tensor_tensor", out=sq[:R, :], in0=ot[:R, :], in1=ot[:R, :], op=ALU.mult)
                    hs = hs_r.next()
                    s.dve("tensor_reduce", out=hs[:R, :], in_=sq[:R, :].rearrange("p (h c) -> p h c", h=4), axis=AX.X, op=ALU.add)
                    self.rms_rstd(hs[:R, :], R, 128)
                    s.dve("tensor_tensor", out=ot[:R, :].rearrange("p (h c) -> p h c", h=4), in0=ot[:R, :].rearrange("p (h c) -> p h c", h=4),
                          in1=hs[:R, :].unsqueeze(2).to_broadcast([R, 4, 128]), op=ALU.mult)
                    s.pool("tensor_tensor", out=ot[:R, :], in0=ot[:R, :], in1=hnw[:R, :], op=ALU.mult)
                    s.dve("tensor_tensor", out=ot[:R, :], in0=ot[:R, :], in1=gt[:R, :], op=ALU.mult)
                    s.dma(STQ, K(self.mix_d[o:o + R, 512:1024], ("mixB", t)), ot[:R, :])
            s.barrier()
        sc.release(m)

    def phase_D(self, l, li):
        s, sc = self.s, self.sc
        m = sc.mark()
        wo = sc.sb([128, 12, D], BF16, name="wo")
        for k3 in range(3):
            s.dma("pool", K(wo[:, 4 * k3:4 * k3 + 4, :], wo.name),
                  self.w_out[l, 512 * k3:512 * (k3 + 1), :].rearrange("(k p) n -> p k n", p=128))
        w2bc = sc.sb([128, D], name="w2bc")
        s.dma("sp", w2bc[:, :], self.norm2_w[l:l + 1, :].to_broadcast([128, D]))
        rw = sc.sb([128, 8, NEXP], name="rw")
        s.dma("sp", rw[:, :, :], self.router_w[l, :, :].rearrange("(k p) e -> p k e", p=128))
        mix_r = sc.sbring(2, [128, 1536], name="mixt")
        psM = sc.psring(1, [128, 1536], name="psM")
        mixT_r = sc.sbring(2, [128, 12, 128], BF16, name="mixT")
        ph_r = sc.psring(2, [128, 512], name="ph")
        ht_r = sc.sbring(2, [128, D], name="ht")
        hn_r = sc.sbring(2, [128, D], name="hn")
        junk = sc.sb([128, D], name="junkd")
        ssq_r = sc.sbring(2, [128, 1], name="ssqd")
        psH = sc.psring(1, [128, 1024], name="psH")
        hnT_r = sc.sbring(2, [128, 8, 128], name="hnT")
        pm_r = sc.psring(1, [128, 512], name="pm")
        sm_r = sc.sbring(2, [128, 4], name="sm")
        ex_r = sc.sbring(2, [128, NEXP], name="ex")
        for t in range(NT):
            R, o = tile_rows(t), tile_off(t)
            mixt = mix_r.next()
            s.dma("sp", mixt[:R, :], K(self.mix_d[o:o + R, :], "mix_all"))
            pM = psM.next()
            for k in range(12):
                s.pe("transpose", out=pM[:, k * 128:k * 128 + R], in_=mixt[:R, k * 128:(k + 1) * 128], identity=self.ident[:R, :R])
            mixT = mixT_r.next()
            s.act("copy", out=mixT[:, :, :R], in_=pM[:, :].rearrange("p (k r) -> p k r", k=12)[:, :, :R])
            ht = ht_r.next()
            s.dma("sp", ht[:R, :], K(self.h_src(li, t), self.hkey(t)))
            for half in range(2):
                ph = ph_r.next()
                for k in range(12):
                    s.pe("matmul", out=ph[:R, :], lhsT=mixT[:, k, :R], rhs=wo[:, k, half * 512:(half + 1) * 512],
                         start=(k == 0), stop=(k == 11))
                s.dve("tensor_tensor", out=K(ht[:R, half * 512:(half + 1) * 512], ht.name),
                      in0=K(ht[:R, half * 512:(half + 1) * 512], ht.name), in1=ph[:R, :], op=ALU.add)
            s.dma(STQ, K(self.y[o:o + R, :], self.hkey(t)), ht[:R, :])
            ssq = ssq_r.next()
            s.act("activation", out=junk[:R, :], in_=ht[:R, :], func=AF.Square, accum_out=ssq[:R, 0:1])
            self.rms_rstd(ssq[:R, 0:1], R, D)
            hn = hn_r.next()
            s.dve("scalar_tensor_tensor", out=hn[:R, :], in0=ht[:R, :], scalar=ssq[:R, 0:1], in1=w2bc[:R, :],
                  op0=ALU.mult, op1=ALU.mult)
            s.dma(STQ, K(self.hn_d[o:o + R, :], ("hn", t)), hn[:R, :])
            pH = psH.next()
            for k in range(8):
                s.pe("transpose", out=pH[:, k * 128:k * 128 + R], in_=hn[:R, k * 128:(k + 1) * 128], identity=self.ident[:R, :R])
            hnT = hnT_r.next()
            s.act("copy", out=hnT[:, :, :R], in_=pH[:, :].rearrange("p (k r) -> p k r", k=8)[:, :, :R])
            pm = pm_r.next()
            for k in range(8):
                s.pe("matmul", out=pm[:R, 0:NEXP], lhsT=hnT[:, k, :R], rhs=rw[:, k, :], start=(k == 0), stop=(k == 7))
            sm = sm_r.next()
            ex = ex_r.next()
            s.dve("tensor_reduce", out=K(sm[:R, 0:1], sm.name), in_=pm[:R, 0:NEXP], axis=AX.X, op=ALU.max)
            s.dve("tensor_scalar", out=K(sm[:R, 1:2], sm.name), in0=K(sm[:R, 0:1], sm.name), scalar1=-1.0, scalar2=None, op0=ALU.mult)
            s.act("activation", out=ex[:R, :], in_=pm[:R, 0:NEXP], func=AF.Exp, bias=K(sm[:R, 1:2], sm.name),
                  accum_out=K(sm[:R, 2:3], sm.name))
            s.dve("reciprocal", out=K(sm[:R, 3:4], sm.name), in_=K(sm[:R, 2:3], sm.name))
            s.dve("tensor_scalar", out=ex[:R, :], in0=ex[:R, :], scalar1=K(sm[:R, 3:4], sm.name), scalar2=None, op0=ALU.mult)
            s.pe("transpose", out=pm[:NEXP, 128:128 + R], in_=ex[:R, :], identity=self.ident[:R, :R])
            s.act("copy", out=K(self.affT[:, o:o + R], self.affT.name), in_=pm[:NEXP, 128:128 + R])
        s.barrier()
        sc.release(m)

    def phase_E(self, l, li):
        s, sc = self.s, self.sc
        m = sc.mark()
        NI = 65
        work = sc.sb([16, L], name="work")
        vals = sc.sb([16, NI * 8], name="vals")
        idxs = sc.sb([16, NI * 8], U32, name="idxs")
        s.dve("tensor_copy", out=work[:, :], in_=self.affT[:, :])
        for i in range(NI):
            v8 = K(vals[:, 8 * i:8 * i + 8], ("vals", i))
            s.dve("max", out=v8, in_=work[:, :])
            s.dve("max_index", out=K(idxs[:, 8 * i:8 * i + 8], ("idxs", i)), in_max=v8, in_values=work[:, :])
            s.dve("match_replace", out=work[:, :], in_to_replace=v8, in_values=work[:, :], imm_value=-1.0)
        s.dma(STQ, K(self.gate_d[:, :], "gate_d"), K(vals[:, :], "valsall"), _r=[("vals", i) for i in range(NI)])
        s.dma(STQ, K(self.idx_d[:, :], "idx_d"), K(idxs[:, :].bitcast(I32), "idxsall"), _r=[("idxs", i) for i in range(NI)])
        s.barrier()
        sc.release(m)

    def phase_F(self, l, li):
        s, sc = self.s, self.sc
        m = sc.mark()
        tiles = [(c * 128, 128) for c in range(4)] + [(512, CAP - 512)]
        ix_r = sc.sbring(2, [128, 5], I32, name="ix")
        gt_r = sc.sbring(2, [128, 5], name="gt")
        xs_r = sc.sbring(2, [128, D], name="xs")
        psX = sc.psring(1, [128, 1024], name="psX")
        xsT_r = sc.sbring(1, [128, 8, 640], BF16, name="xsT")
        wg_r = sc.sbring(2, [128, 8, 512], BF16, name="wg")
        wu_r = sc.sbring(2, [128, 8, 512], BF16, name="wu")
        wd_r = sc.sbring(2, [128, 4, D], BF16, name="wd")
        pA_r = sc.psring(1, [128, 512], name="pA")
        pU_r = sc.psring(1, [128, 512], name="pU")
        pS_r = sc.psring(1, [128, 512], name="pS")
        pD_r = sc.psring(3, [128, 512], name="pD")
        sl_r = sc.sbring(2, [128, 512], name="sl")
        hT_r = sc.sbring(2, [128, 4, 640], BF16, name="hT")
        yacc_r = sc.sbring(1, [128, 5, D], name="yacc")
        for e in range(NEXP):
            ix = ix_r.next()
            gt = gt_r.next()
            s.dma("sp", K(ix[:, 0:4], ix.name), K(self.idx_d[e, 0:512].rearrange("(c p) -> p c", p=128), "idx_d"),
                  allow_slow_non_contiguous=True)
            s.dma("sp", K(ix[0:2, 4:5], ix.name), K(self.idx_d[e:e + 1, 512:514].rearrange("o p -> p o"), "idx_d"),
                  allow_slow_non_contiguous=True)
            s.dma("sp", K(gt[:, 0:4], gt.name), K(self.gate_d[e, 0:512].rearrange("(c p) -> p c", p=128), "gate_d"),
                  allow_slow_non_contiguous=True)
            s.dma("sp", K(gt[0:2, 4:5], gt.name), K(self.gate_d[e:e + 1, 512:514].rearrange("o p -> p o"), "gate_d"),
                  allow_slow_non_contiguous=True)
            xsT = xsT_r.next()
            for c, (co, n) in enumerate(tiles):
                xs = xs_r.next()
                s.dma("pool", xs[:n, :], K(self.hn_d[:, :], "hn_all"), indirect=True,
                      _r=[ix.name], out_offset=None,
                      in_offset=bass.IndirectOffsetOnAxis(ap=ix[:n, c:c + 1], axis=0))
                pX = psX.next()
                for k in range(8):
                    s.pe("transpose", out=pX[:, k * 128:k * 128 + n], in_=xs[:n, k * 128:(k + 1) * 128], identity=self.ident[:n, :n])
                s.act("copy", out=K(xsT[:, :, co:co + n], xsT.name), in_=pX[:, :].rearrange("p (k r) -> p k r", k=8)[:, :, :n])
            yacc = yacc_r.next()
            for gi in range(4):
                wg, wu, wd = wg_r.next(), wu_r.next(), wd_r.next()
                s.dma("poolw", wg[:, :, :], self.w_gate[l, e, :, gi * 512:(gi + 1) * 512].rearrange("(k p) f -> p k f", p=128))
                s.dma("poolw", wu[:, :, :], self.w_up[l, e, :, gi * 512:(gi + 1) * 512].rearrange("(k p) f -> p k f", p=128))
                s.dma("poolw", wd[:, :, :], self.w_down[l, e, gi * 512:(gi + 1) * 512, :].rearrange("(c p) d -> p c d", p=128))
                hT = hT_r.next()
                for c4 in range(4):
                    pA, pU, pS = pA_r.next(), pU_r.next(), pS_r.next()
                    for (w_, p_, so) in ((wg, pA, 0), (wu, pU, 8)):
                        for k in range(8):
                            s.pe("matmul", out=p_[:, :], lhsT=w_[:, k, c4 * 128:(c4 + 1) * 128], rhs=xsT[:, k, 0:512],
                                 start=(k == 0), stop=(k == 7))
                        for k in range(8):
                            s.pe("matmul", out=pS[:, so:so + 2], lhsT=w_[:, k, c4 * 128:(c4 + 1) * 128], rhs=xsT[:, k, 512:514],
                                 start=(k == 0), stop=(k == 7))
                    sl = sl_r.next()
                    s.act("activation", out=sl[:, :], in_=pA[:, :], func=AF.Silu)
                    s.dve("tensor_tensor", out=K(hT[:, c4, 0:512], hT.name), in0=sl[:, :], in1=pU[:, :], op=ALU.mult)
                    sl2 = sl_r.next()
                    s.act("activation", out=sl2[:, 0:2], in_=pS[:, 0:2], func=AF.Silu)
                    s.dve("tensor_tensor", out=K(hT[:, c4, 512:514], hT.name), in0=sl2[:, 0:2], in1=pS[:, 8:10], op=ALU.mult)
                for c, (co, n) in enumerate(tiles):
                    for half in range(2):
                        pD = pD_r.next()
                        for c4 in range(4):
                            s.pe("matmul", out=pD[:n, :], lhsT=hT[:, c4, co:co + n], rhs=wd[:, c4, half * 512:(half + 1) * 512],
                                 start=(c4 == 0), stop=(c4 == 3))
                        ya = K(yacc[:n, c, half * 512:(half + 1) * 512], (yacc.name, c))
                        if gi == 0:
                            s.act("copy", out=ya, in_=pD[:n, :])
                        else:
                            s.dve("tensor_tensor", out=ya, in0=ya, in1=pD[:n, :], op=ALU.add)
            for c, (co, n) in enumerate(tiles):
                ya = K(yacc[:n, c, :], (yacc.name, c))
                s.dve("tensor_scalar", out=ya, in0=ya, scalar1=K(gt[:n, c:c + 1], gt.name), scalar2=None, op0=ALU.mult)
                s.dma("pool", K(self.y[:, :], "y_acc"), ya, indirect=True, _r=[ix.name],
                      out_offset=bass.IndirectOffsetOnAxis(ap=ix[:n, c:c + 1], axis=0), in_offset=None,
                      compute_op=ALU.add)
        s.barrier()
        sc.release(m)

    def build(self):
        s = self.s
        self.load_consts()
        for li, l in enumerate(self.layers):
            ph = self.phases
            if ph == "all" or "A1" in ph:
                self.phase_A1(l, li)
            if ph == "all" or "B1" in ph:
                self.phase_B1(l, li)
            if ph == "all" or "A2" in ph:
                self.phase_A2(l, li)
            if ph == "all" or "B2" in ph:
                self.phase_B2(l, li)
            if ph == "all" or "A3" in ph:
                self.phase_A3(l, li)
            if ph == "all" or "B3a" in ph:
                self.phase_B3a(l, li)
            if ph == "all" or "B3b" in ph:
                self.phase_B3b(l, li)
            if "MIXIN" in ph:
                mixin = self.nc.dram_tensor("mixin", [L, 1536], F32, kind="ExternalInput").ap()
                self._in["mixin"] = mixin
                s.dma("sp", K(self.mix_d[:, :], "mix_all"), K(mixin[:, :], "mixin"))
                s.barrier()
            if ph == "all" or "D" in ph:
                self.phase_D(l, li)
            if ph == "all" or "E" in ph:
                self.phase_E(l, li)
            if ph == "all" or "F" in ph:
                self.phase_F(l, li)
        for name in self.debug:
            src = getattr(self, name)
            dst = self.nc.dram_tensor("dbg_" + name, list(src.shape), F32, kind="ExternalOutput").ap()
            s.dma("sp", K(dst, "dbgout"), K(src, "dbgsrc"))
        s.finish()
        return self.nc


def host_consts():
    ident = np.eye(128, dtype=np.float32)
    n_pair = 16
    inv = (10000.0 ** (-np.arange(n_pair, dtype=np.float32) / n_pair)).astype(np.float32)
    row = np.repeat(np.arange(SEQ // 64), 64).astype(np.float32)
    col = (np.arange(SEQ) % 64).astype(np.float32)
    ang = np.concatenate([row[:, None] * inv, col[:, None] * inv], axis=-1)
    ang = np.concatenate([np.zeros((NMETA, 32), np.float32), ang], axis=0).astype(np.float32)
    cs = np.concatenate([np.cos(ang), np.sin(ang)], axis=-1).astype(np.float32)
    masks = np.zeros((128, 8, 128), np.float32)
    jj, ii = np.meshgrid(np.arange(128), np.arange(128), indexing="ij")
    masks[:, 0, :] = (jj <= ii)
    masks[:, 1, :] = (jj >= ii)
    masks[:, 2, :] = np.where(jj <= ii, 0.0, -30000.0)
    masks[:, 3, :] = np.where(jj >= ii, 0.0, -30000.0)
    masks[:, 4, :] = (jj <= ii).astype(np.float32) - (jj <= 63)
    masks[:, 5, :] = (jj >= ii).astype(np.float32) - (jj >= 64)
    masks[:, 6, :] = (jj <= ii).astype(np.float32) - 1.0
    masks[:, 7, :] = (jj >= ii).astype(np.float32) - 1.0
    return dict(c_ident=ident, c_cs=cs, c_masks=masks)


PARAM_NAMES = ["meta_tokens", "norm1_w", "w_in", "q_norm_w", "k_norm_w", "attn_norm_w", "hgrn_lb", "hgrn_norm_w",
               "conv_w", "conv_b", "dt_bias", "a_log", "d_skip", "ssm_norm_w", "w_out", "norm2_w", "router_w",
               "w_gate", "w_up", "w_down"]


def make_in_maps(inputs, cores, used=None):
    consts = host_consts()
    shared = {}
    for n in PARAM_NAMES:
        a = np.ascontiguousarray(inputs[n], dtype=np.float32)
        if n in ("dt_bias", "a_log"):
            a = a.reshape(DEPTH, 16)
        shared[n] = a
    shared.update(consts)
    maps = []
    for c in cores:
        m = dict(shared)
        m["x"] = np.ascontiguousarray(inputs["x"][c], dtype=np.float32)
        if used is not None:
            m = {k: v for k, v in m.items() if k in used}
        maps.append(m)
    return maps


def kernel(**inputs):
    mk = MK()
    nc = mk.build()
    maps = make_in_maps(inputs, list(range(8)), used=set(mk._in))
    res = run_bass_kernel_spmd(nc, maps, core_ids=list(range(8)))
    out = np.stack([np.asarray(r["y"])[NMETA:] for r in res.results], axis=0)
    return out.astype(np.float32)
```

```python
import numpy as np
import concourse.bass as bass
import concourse.mybir as mybir
from concourse.bass_utils import run_bass_kernel_spmd

F32 = mybir.dt.float32
BF16 = mybir.dt.bfloat16
I32 = mybir.dt.int32
U32 = mybir.dt.uint32
AF = mybir.ActivationFunctionType
ALU = mybir.AluOpType
AX = mybir.AxisListType

D = 1024
SEQ = 4096
NMETA = 16
L = SEQ + NMETA
DEPTH = 4
EPS = 1e-6
INW = 4624
NEXP = 16
FF = 2048
CAP = 2 * L // NEXP
NT = 33
import os
STQ = os.environ.get("MK_STQ", "pool")
MOE_DEPTH = DEPTH


def tile_rows(t):
    return NMETA if t == 0 else 128


def tile_off(t):
    return 0 if t == 0 else NMETA + 128 * (t - 1)


class K:
    def __init__(self, ap, key):
        self.ap = ap
        self.key = key


WRITE_ARGS = ("out", "accum_out", "out_max", "out_indices")
PSUM_KEYS = set()


class Sched:
    def __init__(self, nc, n_dma=12):
        self.nc = nc
        self.eng = {"pe": nc.tensor, "act": nc.scalar, "dve": nc.vector, "pool": nc.gpsimd, "sp": nc.sync}
        self.sem = {}
        self.val = {}
        for e in ("pe", "act", "dve", "pool"):
            self.sem[e] = nc.semaphore("s_" + e).__enter__()
            self.val[e] = 0
        self.dq = {}
        self.qeng = {"sp": "sp", "pool": "pool", "act": "act", "poolw": "pool"}
        for q in ("sp", "pool", "act", "poolw"):
            ids = []
            for i in range(n_dma if q != "poolw" else 3):
                tid = "d_%s%d" % (q, i)
                self.sem[tid] = nc.semaphore(tid).__enter__()
                self.val[tid] = 0
                ids.append(tid)
            self.dq[q] = [ids, 0]
        self.seen = {e: {} for e in self.eng}
        self.res = {}
        self.ninst = 0
        for h in self.sem.values():
            nc.gpsimd.sem_clear(h)
        nc.all_engine_barrier()

    def finish(self):
        self.barrier()
        self.nc.all_engine_barrier()
        for h in self.sem.values():
            self.nc.gpsimd.sem_clear(h)
        self.nc.all_engine_barrier()

    @staticmethod
    def key(x):
        if isinstance(x, K):
            return x.key
        if isinstance(x, (str, tuple)):
            return x
        return x.name

    def _wait(self, e, tl, v):
        if e == "pe" and tl == "pe":
            return
        if self.seen[e].get(tl, 0) >= v:
            return
        self.eng[e].wait_ge(self.sem[tl], v)
        self.seen[e][tl] = v
        self.ninst += 1

    def _deps(self, e, reads, writes):
        for k in reads:
            r = self.res.get(k)
            if r is not None and r[0] is not None:
                self._wait(e, *r[0])
            if r is not None and k in PSUM_KEYS:
                for tl, v in r[1].items():
                    if tl != e:
                        self._wait(e, tl, v)
        for k in writes:
            r = self.res.get(k)
            if r is not None:
                if r[0] is not None:
                    self._wait(e, *r[0])
                for tl, v in r[1].items():
                    self._wait(e, tl, v)

    def _commit(self, tl, v, reads, writes):
        for k in reads:
            r = self.res.setdefault(k, [None, {}])
            if r[1].get(tl, 0) < v:
                r[1][tl] = v
        for k in writes:
            self.res[k] = [(tl, v), {}]

    def _split(self, kw, extra_r, extra_w):
        reads, writes, real = [], [], {}
        for name, v in kw.items():
            if isinstance(v, K):
                (writes if name in WRITE_ARGS else reads).append(v.key)
                real[name] = v.ap
            elif isinstance(v, bass.AP):
                (writes if name in WRITE_ARGS else reads).append(v.name)
                real[name] = v
            else:
                real[name] = v
        reads += [self.key(x) for x in extra_r]
        writes += [self.key(x) for x in extra_w]
        return reads, writes, real

    def op(self, e, name, _r=(), _w=(), **kw):
        reads, writes, real = self._split(kw, _r, _w)
        self._deps(e, reads, writes)
        ins = getattr(self.eng[e], name)(**real)
        self.val[e] += 1
        ins.then_inc(self.sem[e], 1)
        self._commit(e, self.val[e], reads, writes)
        self.ninst += 1
        return ins

    def dma(self, q, out, in_, _r=(), _w=(), indirect=False, **kw):
        reads, writes, real = self._split(dict(out=out, in_=in_), _r, _w)
        ids, pos = self.dq[q]
        tid = ids[pos % len(ids)]
        self.dq[q][1] += 1
        q = self.qeng[q]
        if self.val[tid] > 0:
            self._wait(q, tid, self.val[tid])
        self._deps(q, reads, writes)
        if indirect:
            ins = self.eng[q].indirect_dma_start(out=real["out"], in_=real["in_"], **kw)
        else:
            ins = self.eng[q].dma_start(out=real["out"], in_=real["in_"], **kw)
        self.val[tid] += 16
        ins.then_inc(self.sem[tid], 16)
        self._commit(tid, self.val[tid], reads, writes)
        self.ninst += 1
        return ins

    def barrier(self):
        for e in self.eng:
            for tl, v in self.val.items():
                if v > 0:
                    if e == "sp" and False:
                        continue
                    if self.seen[e].get(tl, 0) < v:
                        self.eng[e].wait_ge(self.sem[tl], v)
                        self.seen[e][tl] = v
                        self.ninst += 1
        self.res.clear()

    def pe(self, name, **kw):
        return self.op("pe", name, **kw)

    def act(self, name, **kw):
        return self.op("act", name, **kw)

    def dve(self, name, **kw):
        return self.op("dve", name, **kw)

    def pool(self, name, **kw):
        return self.op("pool", name, **kw)


class Ring:
    def __init__(self, items):
        self.items = items
        self.i = 0

    def next(self):
        x = self.items[self.i % len(self.items)]
        self.i += 1
        return x


class Scope:
    def __init__(self, nc):
        self.nc = nc
        self.stack = []
        self.n = 0

    def sb(self, shape, dt=F32, name=None):
        self.n += 1
        g = self.nc.sbuf_tensor("%s_%d" % (name or "sb", self.n), list(shape), dt)
        t = g.__enter__()
        self.stack.append(g)
        return t

    def ps(self, shape, dt=F32, name=None):
        self.n += 1
        g = self.nc.psum_tensor("%s_%d" % (name or "ps", self.n), list(shape), dt)
        t = g.__enter__()
        PSUM_KEYS.add(t.name)
        self.stack.append(g)
        return t

    def sbring(self, n, shape, dt=F32, name=None):
        return Ring([self.sb(shape, dt, name) for _ in range(n)])

    def psring(self, n, shape, dt=F32, name=None):
        return Ring([self.ps(shape, dt, name) for _ in range(n)])

    def mark(self):
        return len(self.stack)

    def release(self, mark):
        while len(self.stack) > mark:
            self.stack.pop().__exit__(None, None, None)


class MK:
    def __init__(self, layers=(0, 1, 2, 3), phases="all", debug=()):
        self.layers = list(layers)
        self.phases = phases
        self.debug = set(debug)
        nc = bass.Bass("TRN2", target_bir_lowering=False)
        self.nc = nc
        self.s = Sched(nc)
        self.sc = Scope(nc)
        di = lambda n, sh, dt=F32: nc.dram_tensor(n, list(sh), dt, kind="ExternalInput").ap()
        dscr = lambda n, sh, dt=F32: nc.dram_tensor(n, list(sh), dt, kind="Internal").ap()
        self._in_shapes = dict(
            x=[SEQ, D], meta_tokens=[NMETA, D], norm1_w=[DEPTH, D], w_in=[DEPTH, D, INW], q_norm_w=[DEPTH, 64],
            k_norm_w=[DEPTH, 64], attn_norm_w=[DEPTH, 512], hgrn_lb=[2, DEPTH, 512], hgrn_norm_w=[DEPTH, 512],
            conv_w=[DEPTH, 768, 7], conv_b=[DEPTH, 768], dt_bias=[DEPTH, 16], a_log=[DEPTH, 16], d_skip=[DEPTH, 8],
            ssm_norm_w=[DEPTH, 512], w_out=[DEPTH, 1536, D], norm2_w=[DEPTH, D], router_w=[DEPTH, D, NEXP],
            w_gate=[MOE_DEPTH, NEXP, D, FF], w_up=[MOE_DEPTH, NEXP, D, FF], w_down=[MOE_DEPTH, NEXP, FF, D],
            c_ident=[128, 128], c_cs=[L, 64], c_masks=[128, 8, 128])
        self._in = {}
        self.y = nc.dram_tensor("y", [L, D], F32, kind="ExternalOutput").ap()
        self.qT_d = dscr("qT_d", [128, 4, L], BF16)
        self.kT_d = dscr("kT_d", [128, L], BF16)
        self.V_d = dscr("V_d", [L, 130], BF16)
        self.mix_d = dscr("mix_d", [L, 1536])
        self.hn_d = dscr("hn_d", [L, D])
        self.zs_d = dscr("zs_d", [L, 512])
        self.hq_d = dscr("hq_d", [L, 512])
        self.hgk_d = dscr("hgk_d", [2, 2, L, 512])
        self.hv_d = dscr("hv_d", [L, 512])
        self.hgate_d = dscr("hgate_d", [L, 512])
        self.yh_d = dscr("yh_d", [L, 512])
        self.dtg_d = dscr("dtg_d", [L, 32])
        self.xbcT_d = dscr("xbcT_d", [128, 6, L + 6])
        self.xcT_d = dscr("xcT_d", [128, 6, L])
        self.ys_d = dscr("ys_d", [L, 512])
        self.gate_d = dscr("gate_d", [16, 520])
        self.idx_d = dscr("idx_d", [16, 520], I32)
        self.sc_persist = self.sc
        self.dbg = {}

    def __getattr__(self, name):
        alias = {"meta": "meta_tokens", "ident_d": "c_ident", "cs_d": "c_cs", "masks_d": "c_masks"}
        n = alias.get(name, name)
        shapes = self.__dict__.get("_in_shapes", {})
        if n in shapes:
            if n not in self._in:
                self._in[n] = self.nc.dram_tensor(n, list(shapes[n]), F32, kind="ExternalInput").ap()
            return self._in[n]
        raise AttributeError(name)

    def h_src(self, l, t):
        R, o = tile_rows(t), tile_off(t)
        if l == 0 and self.layers[0] == 0:
            return self.meta[0:R, :] if t == 0 else self.x[o - NMETA:o - NMETA + R, :]
        return self.y[o:o + R, :]

    def hkey(self, t):
        return ("h", t)

    def load_consts(self):
        s, sc = self.s, self.sc
        self.ident = sc.sb([128, 128], name="ident")
        s.dma("sp", self.ident[:, :], self.ident_d[:, :])
        self.masks = sc.sb([128, 8, 128], name="masks")
        s.dma("sp", self.masks[:, :, :], self.masks_d[:, :, :])
        self.ones = sc.sb([128, 128], name="ones")
        self.affT = sc.sb([16, L], name="affT")
        s.pool("memset", ap=K(self.ones[:, :], self.ones.name), constant=1.0, _w=[self.ones.name])

    def rms_rstd(self, ssq, R, n, out=None):
        s = self.s
        s.dve("tensor_scalar", out=ssq, in0=ssq, scalar1=1.0 / n, scalar2=EPS, op0=ALU.mult, op1=ALU.add)
        s.act("activation", out=ssq, in_=ssq, func=AF.Ln)
        s.act("activation", out=ssq, in_=ssq, func=AF.Exp, scale=-0.5)

    def norm_transpose(self, l, li, t, rings, wbc):
        s = self.s
        R = tile_rows(t)
        xt = rings["xt"].next()
        s.dma("sp", xt[:R, :], K(self.h_src(li, t), self.hkey(t)))
        junk = rings["junk"].next()
        ssq = rings["ssq"].next()
        s.act("activation", out=junk[:R, :], in_=xt[:R, :], func=AF.Square, accum_out=ssq[:R, 0:1])
        self.rms_rstd(ssq[:R, 0:1], R, D)
        u = rings["u"].next()
        s.dve("scalar_tensor_tensor", out=u[:R, :], in0=xt[:R, :], scalar=ssq[:R, 0:1], in1=wbc[:R, :],
              op0=ALU.mult, op1=ALU.mult)
        psT = rings["psT"].next()
        for k in range(8):
            s.pe("transpose", out=psT[:, k * 128:k * 128 + R], in_=u[:R, k * 128:(k + 1) * 128],
                 identity=self.ident[:R, :R])
        uT = rings["uT"].next()
        pv = psT[:, :].rearrange("p (k r) -> p k r", k=8)
        s.act("copy", out=uT[:, :, :R], in_=pv[:, :, :R])
        return uT

    def mk_norm_rings(self, sc):
        return dict(
            xt=sc.sbring(2, [128, D], name="xt"), junk=sc.sbring(1, [128, D], name="junk"),
            ssq=sc.sbring(2, [128, 1], name="ssq"), u=sc.sbring(2, [128, D], name="u"),
            psT=sc.psring(1, [128, 1024], name="psT"), uT=sc.sbring(2, [128, 8, 128], BF16, name="uT"))

    def phase_A1(self, l, li):
        s, sc = self.s, self.sc
        m = sc.mark()
        wA = sc.sb([128, 8, 768], BF16, name="wA")
        s.dma("pool", wA[:, :, :], self.w_in[l, :, 0:768].rearrange("(k p) n -> p k n", p=128))
        w1bc = sc.sb([128, D], name="w1bc")
        s.dma("sp", w1bc[:, :], self.norm1_w[l:l + 1, :].to_broadcast([128, D]))
        qkw = sc.sb([128, 10, 64], name="qkw")
        s.dma("sp", K(qkw[:, 0:8, :], qkw.name), self.q_norm_w[l:l + 1, :].unsqueeze(1).to_broadcast([128, 8, 64]))
        s.dma("sp", K(qkw[:, 8:10, :], qkw.name), self.k_norm_w[l:l + 1, :].unsqueeze(1).to_broadcast([128, 2, 64]))
        rings = self.mk_norm_rings(sc)
        psQ = sc.psring(2, [128, 512], name="psQ")
        psKV = sc.psring(2, [128, 512], name="psKV")
        qk_r = sc.sbring(2, [128, 10, 64], name="qk")
        sq_r = sc.sbring(1, [128, 10, 64], name="sq")
        hs_r = sc.sbring(2, [128, 10], name="hs")
        vt_r = sc.sbring(2, [128, 2, 65], BF16, name="vt")
        cs_r = sc.sbring(2, [128, 64], name="cs")
        tmp_r = sc.sbring(4, [128, 8, 32], name="rt")
        qrp_r = sc.sbring(2, [128, 5, 128], name="qrp")
        psT2 = sc.psring(1, [128, 5 * 128], name="psT2")
        qkT_r = sc.sbring(2, [128, 5, 128], BF16, name="qkT")
        import os
        cut = int(os.environ.get("MK_CUT", "99"))
        for t in range(int(os.environ.get("MK_NT", NT))):
            R, o = tile_rows(t), tile_off(t)
            uT = self.norm_transpose(l, li, t, rings, w1bc)
            if cut <= 1:
                continue
            pq = psQ.next()
            pkv = psKV.next()
            for k in range(8):
                s.pe("matmul", out=pq[:R, :], lhsT=uT[:, k, :R], rhs=wA[:, k, 0:512], start=(k == 0), stop=(k == 7))
            for k in range(8):
                s.pe("matmul", out=pkv[:R, 0:256], lhsT=uT[:, k, :R], rhs=wA[:, k, 512:768], start=(k == 0), stop=(k == 7))
            qk = qk_r.next()
            qkf = qk[:, :, :].rearrange("p h d -> p (h d)")
            s.act("copy", out=K(qkf[:R, 0:512], qk.name), in_=pq[:R, :])
            s.act("copy", out=K(qkf[:R, 512:640], qk.name), in_=pkv[:R, 0:128])
            if cut <= 2:
                continue
            vt = vt_r.next()
            s.dve("tensor_copy", out=K(vt[:R, :, 0:64], vt.name), in_=pkv[:R, 128:256].rearrange("p (g d) -> p g d", g=2))
            s.pool("memset", ap=K(vt[:R, :, 64:65], vt.name), constant=1.0, _w=[vt.name])
            s.dma(STQ, K(self.V_d[o:o + R, :], ("V", t)), vt[:R, :, :].rearrange("p g d -> p (g d)"))
            if cut <= 3:
                continue
            sq = sq_r.next()
            s.pool("tensor_tensor", out=sq[:R, :, :], in0=qk[:R, :, :], in1=qk[:R, :, :], op=ALU.mult)
            hs = hs_r.next()
            s.dve("tensor_reduce", out=hs[:R, :], in_=sq[:R, :, :], axis=AX.X, op=ALU.add)
            self.rms_rstd(hs[:R, :], R, 64)
            s.dve("tensor_tensor", out=qk[:R, :, :], in0=qk[:R, :, :], in1=hs[:R, :].unsqueeze(2).to_broadcast([R, 10, 64]), op=ALU.mult)
            s.pool("tensor_tensor", out=qk[:R, :, :], in0=qk[:R, :, :], in1=qkw[:R, :, :], op=ALU.mult)
            if cut <= 4:
                continue
            cs = cs_r.next()
            s.dma("sp", cs[:R, :], self.cs_d[o:o + R, :])
            qrp = qrp_r.next()
            for (h0, nh, oview) in ((0, 8, qrp[:R, 0:4, :].rearrange("p s (g d) -> p g s d", g=2)),
                                   (8, 2, qrp[:R, 4:5, :].rearrange("p s (g d) -> p g s d", g=2))):
                if nh == 8:
                    xin = qk[:R, 0:8, :].rearrange("p (g r) d -> p g r d", g=2)
                    nr = 4
                else:
                    xin = qk[:R, 8:10, :].rearrange("p (g r) d -> p g r d", g=2)
                    nr = 1
                cb = cs[:R, 0:32].unsqueeze(1).unsqueeze(1).to_broadcast([R, 2, nr, 32])
                sb_ = cs[:R, 32:64].unsqueeze(1).unsqueeze(1).to_broadcast([R, 2, nr, 32])
                x1, x2 = xin[:, :, :, 0:32], xin[:, :, :, 32:64]
                t1, t2, t3, t4 = (tmp_r.next() for _ in range(4))
                v = lambda tt: tt[:R, 0:2 * nr, :].rearrange("p (g r) d -> p g r d", g=2)
                s.dve("tensor_tensor", out=K(v(t1), t1.name), in0=K(x1, qk.name), in1=K(cb, cs.name), op=ALU.mult)
                s.pool("tensor_tensor", out=K(v(t2), t2.name), in0=K(x2, qk.name), in1=K(sb_, cs.name), op=ALU.mult)
                s.dve("tensor_tensor", out=K(oview[:, :, :, 0:32], qrp.name), in0=K(v(t1), t1.name), in1=K(v(t2), t2.name), op=ALU.subtract)
                s.pool("tensor_tensor", out=K(v(t3), t3.name), in0=K(x1, qk.name), in1=K(sb_, cs.name), op=ALU.mult)
                s.dve("tensor_tensor", out=K(v(t4), t4.name), in0=K(x2, qk.name), in1=K(cb, cs.name), op=ALU.mult)
                s.pool("tensor_tensor", out=K(oview[:, :, :, 32:64], qrp.name), in0=K(v(t3), t3.name), in1=K(v(t4), t4.name), op=ALU.add)
            if cut <= 5:
                continue
            pT = psT2.next()
            for j in range(5):
                s.pe("transpose", out=pT[:, j * 128:j * 128 + R], in_=qrp[:R, j, :], identity=self.ident[:R, :R])
            qkT = qkT_r.next()
            s.act("copy", out=qkT[:, :, :R], in_=pT[:, :].rearrange("p (j r) -> p j r", j=5)[:, :, :R])
            s.dma(STQ, K(self.qT_d[:, :, o:o + R], ("qT", t)), qkT[:, 0:4, :R])
            s.dma(STQ, K(self.kT_d[:, o:o + R], ("kT", t)), qkT[:, 4, :R])
        s.barrier()
        sc.release(m)

    def phase_B1(self, l, li):
        s, sc = self.s, self.sc
        m = sc.mark()
        kT = sc.sb([128, L], BF16, name="kT")
        s.dma("sp", kT[:, :], K(self.kT_d[:, :], "kT_all"), _r=[("kT", t) for t in range(NT)])
        V = sc.sb([128, NT, 130], BF16, name="V")
        for q4 in range(4):
            s.dma("sp", K(V[:, 8 * q4:8 * q4 + 8, :], V.name),
                  K(self.V_d[NMETA + 1024 * q4:NMETA + 1024 * (q4 + 1), :].rearrange("(t p) c -> p t c", p=128), "V_all"),
                  _r=[("V", t) for t in range(NT)])
        s.dma("sp", K(V[0:NMETA, 32, :], V.name), K(self.V_d[0:NMETA, :], "V_all"))
        anw = sc.sb([128, 512], name="anw")
        s.dma("sp", anw[:, :], self.attn_norm_w[l:l + 1, :].to_broadcast([128, 512]))
        qT_r = sc.sbring(2, [128, 4, 512], BF16, name="qTb")
        psS = sc.psring(2, [128, 1024], name="psS")
        pT_r = sc.sbring(3, [128, 1024], BF16, name="pT")
        psO = sc.psring(2, [128, 512], name="psO")
        oT_r = sc.sbring(2, [128, 512], name="oT")
        psB = sc.psring(2, [128, 512], name="psB")
        att_r = sc.sbring(2, [128, 4, 512], name="att")
        rc_r = sc.sbring(2, [128, 1], name="rc")
        junk = sc.sb([128, 512], name="junkb")
        ssq_r = sc.sbring(2, [128, 1], name="ssqb")
        qblocks = [(0, NMETA)] + [(NMETA + 512 * b, 512) for b in range(8)]
        ksteps = [[(2 * i, NMETA + 256 * i, 128), (2 * i + 1, NMETA + 256 * i + 128, 128)] for i in range(16)] + [[(32, 0, NMETA)]]
        for (qo, NQ) in qblocks:
            qT = qT_r.next()
            s.dma("sp", qT[:, :, :NQ], K(self.qT_d[:, :, qo:qo + NQ], "qT_all"), _r=[("qT", t) for t in range(NT)])
            att = att_r.next()
            nsub = (NQ + 127) // 128
            for g in range(2):
                for r in range(4):
                    h = g * 4 + r
                    po = psO.next()
                    def exp_pv(si, step, ps):
                        pT = pT_r.next()
                        KR = step[0][2]
                        if len(step) == 2 and NQ == 512:
                            s.act("activation", out=pT[:KR, :], in_=ps[:KR, :], func=AF.Exp, scale=0.125)
                        else:
                            for j in range(len(step)):
                                s.act("activation", out=pT[:KR, j * 512:j * 512 + NQ], in_=ps[:KR, j * 512:j * 512 + NQ],
                                      func=AF.Exp, scale=0.125)
                        for j, (kt, ko, KR) in enumerate(step):
                            s.pe("matmul", out=po[:65, :NQ], lhsT=V[:KR, kt, g * 65:(g + 1) * 65], rhs=pT[:KR, j * 512:j * 512 + NQ],
                                 start=(si == 0 and j == 0), stop=(si == len(ksteps) - 1))
                    prev = None
                    for si, step in enumerate(ksteps):
                        ps = psS.next()
                        for j, (kt, ko, KR) in enumerate(step):
                            s.pe("matmul", out=ps[:KR, j * 512:j * 512 + NQ], lhsT=kT[g * 64:(g + 1) * 64, ko:ko + KR],
                                 rhs=qT[g * 64:(g + 1) * 64, r, :NQ], start=True, stop=True)
                        if prev is not None:
                            exp_pv(*prev)
                        prev = (si, step, ps)
                    exp_pv(*prev)
                    oT = oT_r.next()
                    s.dve("tensor_copy", out=oT[:65, :NQ], in_=po[:65, :NQ])
                    pb = psB.next()
                    for sb_ in range(nsub):
                        n = min(128, NQ - sb_ * 128)
                        s.pe("transpose", out=pb[:n, sb_ * 65:sb_ * 65 + 65], in_=oT[:65, sb_ * 128:sb_ * 128 + n],
                             identity=self.ident[:65, :65])
                    for sb_ in range(nsub):
                        n = min(128, NQ - sb_ * 128)
                        rc = rc_r.next()
                        s.dve("reciprocal", out=rc[:n, :], in_=pb[:n, sb_ * 65 + 64:sb_ * 65 + 65])
                        s.dve("tensor_scalar", out=K(att[:n, sb_, h * 64:(h + 1) * 64], att.name),
                              in0=pb[:n, sb_ * 65:sb_ * 65 + 64], scalar1=rc[:n, 0:1], scalar2=None, op0=ALU.mult)
            for sb_ in range(nsub):
                n = min(128, NQ - sb_ * 128)
                ssq = ssq_r.next()
                s.act("activation", out=junk[:n, :], in_=K(att[:n, sb_, :], att.name), func=AF.Square, accum_out=ssq[:n, 0:1])
                self.rms_rstd(ssq[:n, 0:1], n, 512)
                s.dve("scalar_tensor_tensor", out=K(att[:n, sb_, :], att.name), in0=K(att[:n, sb_, :], att.name),
                      scalar=ssq[:n, 0:1], in1=anw[:n, :], op0=ALU.mult, op1=ALU.mult)
                o = qo + sb_ * 128
                s.dma(STQ, K(self.mix_d[o:o + n, 0:512], ("mixA", o)), K(att[:n, sb_, :], att.name))
        s.barrier()
        sc.release(m)

    def phase_A3(self, l, li):
        s, sc = self.s, self.sc
        m = sc.mark()
        wS = sc.sb([128, 8, 1296], BF16, name="wS")
        s.dma("pool", wS[:, :, :], self.w_in[l, :, 3328:4624].rearrange("(k p) n -> p k n", p=128))
        w1bc = sc.sb([128, D], name="w1bc")
        s.dma("sp", w1bc[:, :], self.norm1_w[l:l + 1, :].to_broadcast([128, D]))
        dtb = sc.sb([128, 16], name="dtb")
        s.dma("sp", dtb[:, :], self.dt_bias[l:l + 1, :].to_broadcast([128, 16]))
        Abc = sc.sb([128, 16], name="Abc")
        s.dma("sp", Abc[:, :], self.a_log[l:l + 1, :].to_broadcast([128, 16]))
        s.act("activation", out=Abc[:, :], in_=Abc[:, :], func=AF.Exp)
        s.dve("tensor_scalar", out=Abc[:, :], in0=Abc[:, :], scalar1=-1.0, scalar2=None, op0=ALU.mult)
        zero = sc.sb([128, 6, 3], name="zero")
        s.dve("memset", ap=zero[:, :, :], constant=0.0, _w=[zero.name])
        s.dma(STQ, K(self.xbcT_d[:, :, 0:3], ("xbcT", "h0")), zero[:, :, :])
        s.dma(STQ, K(self.xbcT_d[:, :, 3 + L:6 + L], ("xbcT", "h1")), zero[:, :, :])
        rings = self.mk_norm_rings(sc)
        pz_r = sc.psring(2, [128, 512], name="pz")
        pd_r = sc.psring(1, [128, 512], name="pd")
        px_r = sc.psring(1, [128, 1024], name="px")
        zs_r = sc.sbring(2, [128, 512], name="zs")
        dtg_r = sc.sbring(2, [128, 32], name="dtg")
        xT_r = sc.sbring(2, [128, 6, 128], name="xTs")
        for t in range(NT):
            R, o = tile_rows(t), tile_off(t)
            uT = self.norm_transpose(l, li, t, rings, w1bc)
            pz = pz_r.next()
            for k in range(8):
                s.pe("matmul", out=pz[:R, :], lhsT=uT[:, k, :R], rhs=wS[:, k, 0:512], start=(k == 0), stop=(k == 7))
            zs = zs_r.next()
            s.act("activation", out=zs[:R, :], in_=pz[:R, :], func=AF.Silu)
            s.dma(STQ, K(self.zs_d[o:o + R, :], ("zs", t)), zs[:R, :])
            pd = pd_r.next()
            for k in range(8):
                s.pe("matmul", out=pd[:R, 0:16], lhsT=uT[:, k, :R], rhs=wS[:, k, 1280:1296], start=(k == 0), stop=(k == 7))
            dtg = dtg_r.next()
            s.dve("tensor_tensor", out=K(dtg[:R, 0:16], dtg.name), in0=pd[:R, 0:16], in1=dtb[:R, :], op=ALU.add)
            s.act("activation", out=K(dtg[:R, 0:16], dtg.name), in_=K(dtg[:R, 0:16], dtg.name), func=AF.Exp)
            s.act("activation", out=K(dtg[:R, 0:16], dtg.name), in_=K(dtg[:R, 0:16], dtg.name), func=AF.Ln, bias=1.0)
            s.dve("tensor_tensor", out=K(dtg[:R, 16:32], dtg.name), in0=K(dtg[:R, 0:16], dtg.name), in1=Abc[:R, :], op=ALU.mult)
            s.dma(STQ, K(self.dtg_d[o:o + R, :], ("dtg", t)), dtg[:R, :])
            px = px_r.next()
            for cc in range(6):
                for k in range(8):
                    s.pe("matmul", out=px[:, cc * 128:cc * 128 + R], lhsT=wS[:, k, 512 + cc * 128:512 + (cc + 1) * 128],
                         rhs=uT[:, k, :R], start=(k == 0), stop=(k == 7))
            xT = xT_r.next()
            s.act("copy", out=xT[:, :, :R], in_=px[:, 0:768].rearrange("p (c r) -> p c r", c=6)[:, :, :R])
            s.dma(STQ, K(self.xbcT_d[:, :, 3 + o:3 + o + R], ("xbcT", t)), xT[:, :, :R])
        s.barrier()
        sc.release(m)

    def phase_B3a(self, l, li):
        s, sc = self.s, self.sc
        m = sc.mark()
        cw = sc.sb([128, 6, 7], name="cw")
        s.dma("sp", cw[:, :, :], self.conv_w[l, :, :].rearrange("(c p) k -> p c k", p=128))
        cb = sc.sb([128, 6], name="cb")
        s.dma("sp", cb[:, :], self.conv_b[l, :].rearrange("(c p) -> p c", p=128), allow_slow_non_contiguous=True)
        xin_r = sc.sbring(2, [128, L + 6], name="xin")
        acc_r = sc.sbring(2, [128, L], name="acc")
        for cc in range(6):
            xin = xin_r.next()
            s.dma("sp", xin[:, :], K(self.xbcT_d[:, cc, :], "xbcT_all"))
            acc = acc_r.next()
            eng = "dve"
            s.op(eng, "tensor_scalar", out=acc[:, :], in0=xin[:, 0:L], scalar1=K(cw[:, cc, 0:1], cw.name),
                 scalar2=K(cb[:, cc:cc + 1], cb.name), op0=ALU.mult, op1=ALU.add)
            for k in range(1, 7):
                s.op(eng, "scalar_tensor_tensor", out=acc[:, :], in0=xin[:, k:k + L], scalar=K(cw[:, cc, k:k + 1], cw.name),
                     in1=acc[:, :], op0=ALU.mult, op1=ALU.add)
            s.act("activation", out=acc[:, :], in_=acc[:, :], func=AF.Silu)
            s.dma(STQ, K(self.xcT_d[:, cc, :], ("xcT", cc)), acc[:, :])
        s.barrier()
        sc.release(m)

    def phase_B3b(self, l, li):
        s, sc = self.s, self.sc
        m = sc.mark()
        dsk = sc.sb([128, 8], name="dsk")
        s.dma("sp", dsk[:, :], self.d_skip[l:l + 1, :].to_broadcast([128, 8]))
        snw = sc.sb([128, 512], name="snw")
        s.dma("sp", snw[:, :], self.ssm_norm_w[l:l + 1, :].to_broadcast([128, 512]))
        S = sc.sb([128, 4, 64], name="Sst")
        xc_r = sc.sbring(2, [128, 6, 128], name="xc")
        dtg_r = sc.sbring(2, [128, 32], name="dtgl")
        pXt_r = sc.psring(1, [128, 1024], name="pXt")
        xtok_r = sc.sbring(2, [128, 640], name="xtok")
        xdt_r = sc.sbring(2, [128, 8, 64], name="xdt")
        pGS_r = sc.psring(1, [128, 512], name="pGS")
        cum_r = sc.sbring(2, [128, 8], name="cum")
        sm_r = sc.sbring(2, [128, 5, 8], name="smv")
        ST_r = sc.sbring(2, [128, 2, 128], name="ST")
        gbc_r = sc.sbring(2, [128, 8, 128], name="gbc")
        pDm_r = sc.psring(2, [128, 512], name="pDm")
        attd_r = sc.sbring(3, [128, 128], name="attd")
        attm_r = sc.sbring(3, [128, 128], name="attm")
        py_r = sc.psring(1, [128, 512], name="py")
        pI_r = sc.psring(1, [128, 512], name="pI")
        pSt_r = sc.psring(1, [128, 512], name="pSt")
        yt_r = sc.sbring(2, [128, 512], name="yt")
        yf_r = sc.sbring(2, [128, 512], name="yfl")
        zs_r = sc.sbring(2, [128, 512], name="zsl")
        xdw_r = sc.sbring(2, [128, 8, 64], name="xdw")
        tmp_r = sc.sbring(2, [128, 4, 64], name="tmps")
        junk = sc.sb([128, 512], name="junks")
        ssq_r = sc.sbring(2, [128, 1], name="ssqs")
        cut3 = int(os.environ.get("MK_B3CUT", "99"))
        nd = int(os.environ.get("MK_B3ND", "2"))
        for d in range(nd):
            s.dve("memset", ap=S[:, :, :], constant=0.0, _w=[S.name])
            order = range(NT) if d == 0 else range(NT - 1, -1, -1)
            order = list(order)[:int(os.environ.get("MK_NT", NT))]
            U = self.masks[:, d, :]
            NEG = self.masks[:, 2 + d, :]
            for t in order:
                R, o = tile_rows(t), tile_off(t)
                xc = xc_r.next()
                s.dma("sp", xc[:, :, :R], K(self.xcT_d[:, :, o:o + R], "xcT_all"))
                dtg = dtg_r.next()
                s.dma("sp", dtg[:R, :], K(self.dtg_d[o:o + R, :], "dtg_all"))
                pXt = pXt_r.next()
                for cc in range(5):
                    s.pe("transpose", out=pXt[:R, cc * 128:(cc + 1) * 128], in_=xc[:, cc, :R], identity=self.ident[:, :])
                xtok = xtok_r.next()
                s.act("copy", out=xtok[:R, :], in_=pXt[:R, 0:640])
                xdt = xdt_r.next()
                xv = xtok[:R, 0:512].rearrange("p (h q) -> p h q", h=8)
                s.dve("tensor_tensor", out=xdt[:R, :, :], in0=K(xv, xtok.name),
                      in1=K(dtg[:R, d * 8:d * 8 + 8].unsqueeze(2).to_broadcast([R, 8, 64]), dtg.name), op=ALU.mult)
                g8 = dtg[:R, 16 + d * 8:16 + d * 8 + 8]
                if cut3 <= 1:
                    continue
                pGS = pGS_r.next()
                s.pe("matmul", out=pGS[:R, 0:8], lhsT=U[:R, :R], rhs=K(g8, dtg.name), start=True, stop=True)
                pTot = pGS[:, 384:392]
                s.pe("matmul", out=pTot[:, 0:8], lhsT=self.ones[:R, :], rhs=K(g8, dtg.name), start=True, stop=True)
                sm = sm_r.next()
                s.act("copy", out=K(sm[:R, 0, :], sm.name), in_=pGS[:R, 0:8])
                s.dve("tensor_scalar", out=K(sm[:R, 1, :], sm.name), in0=K(sm[:R, 0, :], sm.name), scalar1=-1.0, scalar2=None, op0=ALU.mult)
                s.dve("tensor_tensor", out=K(sm[:R, 2, :], sm.name), in0=pTot[:R, 0:8], in1=K(sm[:R, 0, :], sm.name), op=ALU.subtract)
                s.act("activation", out=K(sm[:R, 2, :], sm.name), in_=K(sm[:R, 2, :], sm.name), func=AF.Exp)
                s.act("activation", out=K(sm[:R, 3, :], sm.name), in_=K(sm[:R, 0, :], sm.name), func=AF.Exp)
                s.act("activation", out=K(sm[:, 4, :], sm.name), in_=pTot[:, 0:8], func=AF.Exp)
                if cut3 <= 2:
                    continue
                pI = pI_r.next()
                stp = (pGS[:R, 128:128 + R], pI[:R, 0:R])
                for g in range(2):
                    s.pe("matmul", out=stp[g], lhsT=xc[g * 64:(g + 1) * 64, 4, :R],
                         rhs=xc[g * 64:(g + 1) * 64, 5, :R], start=True, stop=True)
                if cut3 == 3 and os.environ.get("MK_B3SUB") == "1":
                    continue
                ST = ST_r.next()
                for g in range(2):
                    s.dve("tensor_tensor", out=K(ST[:R, g, :R], ST.name), in0=stp[g], in1=K(U[:R, :R], self.masks.name), op=ALU.mult)
                if cut3 == 3 and os.environ.get("MK_B3SUB") == "2":
                    continue
                gbc = gbc_r.next()
                s.dve("tensor_tensor", out=gbc[:R, :, :R], in0=K(self.ones[:R, :R].unsqueeze(1).to_broadcast([R, 8, R]), self.ones.name),
                       in1=K(g8.unsqueeze(2).to_broadcast([R, 8, R]), dtg.name), op=ALU.mult)
                if cut3 <= 3:
                    continue
                py = py_r.next()
                for h in range(8):
                    g = h // 4
                    pDm = pDm_r.next()
                    s.pe("matmul", out=pDm[:R, :R], lhsT=gbc[:R, h, :R], rhs=K(U[:R, :R], self.masks.name), start=True, stop=False)
                    s.pe("matmul", out=pDm[:R, :R], lhsT=self.ident[:R, :R], rhs=K(NEG[:R, :R], self.masks.name), start=False, stop=True)
                    attd = attd_r.next()
                    s.act("activation", out=attd[:R, :R], in_=pDm[:R, :R], func=AF.Exp, bias=K(sm[:R, 1, h:h + 1], sm.name))
                    attm = attm_r.next()
                    s.pool("tensor_tensor", out=attm[:R, :R], in0=attd[:R, :R], in1=K(ST[:R, g, :R], ST.name), op=ALU.mult)
                    s.pe("matmul", out=py[:R, h * 64:(h + 1) * 64], lhsT=attm[:R, :R], rhs=K(xdt[:R, h, :], xdt.name), start=True, stop=True)
                if cut3 <= 4:
                    continue
                ip = (pI[:R, 0:256], pGS[:R, 0:256])
                for g in range(2):
                    s.pe("matmul", out=ip[g], lhsT=xc[g * 64:(g + 1) * 64, 5, :R],
                         rhs=S[g * 64:(g + 1) * 64, :, :].rearrange("p r q -> p (r q)"), start=True, stop=True)
                yt = yt_r.next()
                for g in range(2):
                    s.dve("tensor_tensor", out=K(yt[:R, g * 256:(g + 1) * 256].rearrange("p (h q) -> p h q", h=4), yt.name),
                          in0=ip[g].rearrange("p (h q) -> p h q", h=4),
                          in1=K(sm[:R, 3, g * 4:g * 4 + 4].unsqueeze(2).to_broadcast([R, 4, 64]), sm.name), op=ALU.mult)
                s.dve("tensor_tensor", out=yt[:R, :], in0=yt[:R, :], in1=py[:R, :], op=ALU.add)
                if cut3 <= 5:
                    continue
                xdw = xdw_r.next()
                s.pool("tensor_tensor", out=xdw[:R, :, :], in0=xdt[:R, :, :],
                       in1=K(sm[:R, 2, :].unsqueeze(2).to_broadcast([R, 8, 64]), sm.name), op=ALU.mult)
                pSt = pSt_r.next()
                s.pe("matmul", out=pSt[:, :], lhsT=xtok[:R, 512:640], rhs=xdw[:R, :, :].rearrange("p h q -> p (h q)"), start=True, stop=True)
                for g in range(2):
                    gs = slice(g * 64, (g + 1) * 64)
                    tmp = tmp_r.next()
                    s.dve("tensor_tensor", out=K(tmp[gs, :, :], tmp.name), in0=K(S[gs, :, :], S.name),
                          in1=K(sm[gs, 4, g * 4:g * 4 + 4].unsqueeze(2).to_broadcast([64, 4, 64]), sm.name), op=ALU.mult)
                    s.dve("tensor_tensor", out=K(S[gs, :, :], S.name), in0=K(tmp[gs, :, :], tmp.name),
                          in1=pSt[gs, g * 256:(g + 1) * 256].rearrange("p (r q) -> p r q", r=4), op=ALU.add)
                if cut3 <= 6:
                    continue
                if d == 0:
                    s.dma(STQ, K(self.ys_d[o:o + R, :], ("ys", t)), yt[:R, :])
                else:
                    yf = yf_r.next()
                    s.dma("sp", yf[:R, :], K(self.ys_d[o:o + R, :], ("ys", t)))
                    zs = zs_r.next()
                    s.dma("sp", zs[:R, :], K(self.zs_d[o:o + R, :], "zs_all"))
                    s.dve("tensor_tensor", out=yt[:R, :], in0=yt[:R, :], in1=yf[:R, :], op=ALU.add)
                    s.pool("tensor_tensor", out=yf[:R, :].rearrange("p (h q) -> p h q", h=8), in0=K(xv, xtok.name),
                           in1=K(dsk[:R, :].unsqueeze(2).to_broadcast([R, 8, 64]), dsk.name), op=ALU.mult)
                    s.dve("tensor_tensor", out=yt[:R, :], in0=yt[:R, :], in1=yf[:R, :], op=ALU.add)
                    s.dve("tensor_tensor", out=yt[:R, :], in0=yt[:R, :], in1=zs[:R, :], op=ALU.mult)
                    ssq = ssq_r.next()
                    s.act("activation", out=junk[:R, :], in_=yt[:R, :], func=AF.Square, accum_out=ssq[:R, 0:1])
                    self.rms_rstd(ssq[:R, 0:1], R, 512)
                    s.dve("scalar_tensor_tensor", out=yt[:R, :], in0=yt[:R, :], scalar=ssq[:R, 0:1], in1=snw[:R, :],
                          op0=ALU.mult, op1=ALU.mult)
                    s.dma(STQ, K(self.mix_d[o:o + R, 1024:1536], ("mixC", t)), yt[:R, :])
            s.barrier()
        sc.release(m)

    def phase_A2(self, l, li):
        s, sc = self.s, self.sc
        m = sc.mark()
        wH = sc.sb([128, 8, 2560], BF16, name="wH")
        for k2 in range(4):
            s.dma("pool", K(wH[:, 2 * k2:2 * k2 + 2, :], wH.name),
                  self.w_in[l, 256 * k2:256 * (k2 + 1), 768:3328].rearrange("(k p) n -> p k n", p=128))
        w1bc = sc.sb([128, D], name="w1bc")
        s.dma("sp", w1bc[:, :], self.norm1_w[l:l + 1, :].to_broadcast([128, D]))
        lbe = sc.sb([128, 2, DEPTH, 512], name="lbe")
        for dd in range(2):
            s.dma("sp", K(lbe[:, dd, :, :], lbe.name), self.hgrn_lb[dd:dd + 1, :, :].to_broadcast([128, DEPTH, 512]))
        s.act("activation", out=lbe[:, :, :, :], in_=lbe[:, :, :, :], func=AF.Exp)
        lsum = sc.sb([128, 2, 512], name="lsum")
        lbb = sc.sb([128, 2, 512], name="lbb")
        s.dve("tensor_tensor", out=lsum[:, :, :], in0=K(lbe[:, :, 0, :], lbe.name), in1=K(lbe[:, :, 1, :], lbe.name), op=ALU.add)
        s.dve("tensor_tensor", out=lsum[:, :, :], in0=lsum[:, :, :], in1=K(lbe[:, :, 2, :], lbe.name), op=ALU.add)
        s.dve("tensor_tensor", out=lsum[:, :, :], in0=lsum[:, :, :], in1=K(lbe[:, :, 3, :], lbe.name), op=ALU.add)
        s.dve("reciprocal", out=lsum[:, :, :], in_=lsum[:, :, :])
        s.dve("memset", ap=lbb[:, :, :], constant=0.0, _w=[lbb.name])
        for lp in range(1, l + 1):
            s.dve("tensor_tensor", out=lbb[:, :, :], in0=lbb[:, :, :], in1=K(lbe[:, :, lp, :], lbe.name), op=ALU.add)
        s.dve("tensor_tensor", out=lbb[:, :, :], in0=lbb[:, :, :], in1=lsum[:, :, :], op=ALU.mult)
        s.dve("tensor_scalar", out=lbb[:, :, :], in0=lbb[:, :, :], scalar1=0.0, scalar2=1.0, op0=ALU.max, op1=ALU.min)
        rings = self.mk_norm_rings(sc)
        pp_r = sc.psring(3, [128, 512], name="pp")
        o_r = sc.sbring(3, [128, 512], name="oA2")
        e_r = sc.sbring(2, [128, 512], name="eA2")
        n_r = sc.sbring(2, [128, 512], name="nA2")
        f_r = sc.sbring(2, [128, 512], name="fA2")
        for t in range(NT):
            R, o = tile_rows(t), tile_off(t)
            uT = self.norm_transpose(l, li, t, rings, w1bc)

            def proj(ci):
                pp = pp_r.next()
                for k in range(8):
                    s.pe("matmul", out=pp[:R, :], lhsT=uT[:, k, :R], rhs=wH[:, k, ci * 512:(ci + 1) * 512],
                         start=(k == 0), stop=(k == 7))
                return pp
            pq = proj(0)
            ot = o_r.next()
            s.act("activation", out=ot[:R, :], in_=pq[:R, :], func=AF.Silu)
            s.dma(STQ, K(self.hq_d[o:o + R, :], ("hq", t)), ot[:R, :])
            for dd in range(2):
                pf = proj(1 + dd)
                e = e_r.next()
                s.dve("tensor_scalar", out=e[:R, :], in0=pf[:R, :], scalar1=-1.0, scalar2=30.0, op0=ALU.mult, op1=ALU.min)
                s.act("activation", out=e[:R, :], in_=e[:R, :], func=AF.Exp)
                num = n_r.next()
                s.dve("tensor_tensor", out=num[:R, :], in0=e[:R, :], in1=K(lbb[:R, dd, :], lbb.name), op=ALU.mult)
                s.pool("tensor_scalar", out=e[:R, :], in0=e[:R, :], scalar1=1.0, scalar2=None, op0=ALU.add)
                s.dve("reciprocal", out=e[:R, :], in_=e[:R, :])
                f = f_r.next()
                s.dve("scalar_tensor_tensor", out=f[:R, :], in0=num[:R, :], scalar=1.0, in1=e[:R, :], op0=ALU.add, op1=ALU.mult)
                s.pool("tensor_scalar", out=f[:R, :], in0=f[:R, :], scalar1=1.0, scalar2=None, op0=ALU.min)
                s.act("activation", out=num[:R, :], in_=f[:R, :], func=AF.Ln)
                s.dma(STQ, K(self.hgk_d[dd, 0, o:o + R, :], ("hg", dd, t)), num[:R, :])
                s.pool("tensor_scalar", out=f[:R, :], in0=f[:R, :], scalar1=-1.0, scalar2=1.0, op0=ALU.mult, op1=ALU.add)
                s.dma(STQ, K(self.hgk_d[dd, 1, o:o + R, :], ("hk", dd, t)), f[:R, :])
            pi = proj(3)
            ot = o_r.next()
            s.act("copy", out=ot[:R, :], in_=pi[:R, :])
            s.dma(STQ, K(self.hv_d[o:o + R, :], ("hv", t)), ot[:R, :])
            pg = proj(4)
            ot = o_r.next()
            s.act("activation", out=ot[:R, :], in_=pg[:R, :], func=AF.Silu)
            s.dma(STQ, K(self.hgate_d[o:o + R, :], ("hgate", t)), ot[:R, :])
        s.barrier()
        sc.release(m)

    def phase_B2(self, l, li):
        s, sc = self.s, self.sc
        m = sc.mark()
        hnw = sc.sb([128, 512], name="hnw")
        s.dma("sp", hnw[:, :], self.hgrn_norm_w[l:l + 1, :].to_broadcast([128, 512]))
        mv = sc.sb([128, 2, 2, 2], name="mv")
        s.dve("memset", ap=mv[:, :, :, :], constant=0.0, _w=[mv.name])
        s.dve("memset", ap=K(mv[0:64, 0, 0, 0:1], mv.name), constant=1.0, _w=[mv.name])
        s.dve("memset", ap=K(mv[64:128, 0, 0, 1:2], mv.name), constant=1.0, _w=[mv.name])
        s.dve("memset", ap=K(mv[64:128, 1, 0, 0:1], mv.name), constant=1.0, _w=[mv.name])
        s.dve("memset", ap=K(mv[0:64, 1, 0, 1:2], mv.name), constant=1.0, _w=[mv.name])
        s.dve("memset", ap=K(mv[:, :, 1, 0:1], mv.name), constant=1.0, _w=[mv.name])
        S = sc.sb([128, 4, 128], name="Sgla")
        Sp = sc.sb([128, 4, 128], name="Spgla")
        q_r = sc.sbring(2, [128, 512], name="qg")
        g_r = sc.sbring(2, [128, 512], name="gg")
        k_r = sc.sbring(2, [128, 512], name="kg")
        v_r = sc.sbring(2, [128, 512], name="vg")
        pE_r = sc.psring(1, [128, 512], name="pE")
        ex_r = sc.sbring(2, [128, 512], name="exg")
        qt_r = sc.sbring(2, [128, 512], name="qtl")
        kt_r = sc.sbring(2, [128, 512], name="ktl")
        pT_r = sc.psring(1, [128, 1024], name="pTg")
        qkT_r = sc.sbring(2, [128, 8, 128], name="qkTg")
        pA_r = sc.psring(2, [128, 512], name="pAg")
        attS_r = sc.sbring(2, [128, 128], name="attS")
        att16_r = sc.sbring(2, [16, 16], name="att16")
        py_r = sc.psring(1, [128, 512], name="pyg")
        pV_r = sc.psring(1, [128, 512], name="pVg")
        ev_r = sc.sbring(2, [128, 4, 3], name="evg")
        pS_r = sc.psring(1, [128, 512], name="pSg")
        o_r = sc.sbring(2, [128, 512], name="og")
        yf_r = sc.sbring(2, [128, 512], name="yfg")
        gt_r = sc.sbring(2, [128, 512], name="gtg")
        sq_r = sc.sbring(1, [128, 512], name="sqg")
        hs_r = sc.sbring(2, [128, 4], name="hsg")
        for d in range(2):
            s.dve("memset", ap=S[:, :, :], constant=0.0, _w=[S.name])
            for a_ in attS_r.items:
                s.dve("memset", ap=a_[:, :], constant=0.0, _w=[a_.name])
            order = range(NT) if d == 0 else range(NT - 1, -1, -1)
            for t in order:
                R, o = tile_rows(t), tile_off(t)
                big = (R == 128)
                W = self.masks[:, 4 + d, :] if big else self.masks[:, 6 + d, :]
                M = self.masks[:, d, :]
                q, g, k, v = q_r.next(), g_r.next(), k_r.next(), v_r.next()
                s.dma("sp", q[:R, :], K(self.hq_d[o:o + R, :], "hq_all"))
                s.dma("sp", g[:R, :], K(self.hgk_d[d, 0, o:o + R, :], "hg_all"))
                s.dma("sp", k[:R, :], K(self.hgk_d[d, 1, o:o + R, :], "hk_all"))
                s.dma("sp", v[:R, :], K(self.hv_d[o:o + R, :], "hv_all"))
                pE = pE_r.next()
                s.pe("matmul", out=pE[:R, :], lhsT=K(W[:R, :R], self.masks.name), rhs=g[:R, :], start=True, stop=True)
                ex1, ex2 = ex_r.next(), ex_r.next()
                s.act("activation", out=ex1[:R, :], in_=pE[:R, :], func=AF.Exp)
                s.act("activation", out=ex2[:R, :], in_=pE[:R, :], func=AF.Exp, scale=-1.0)
                qt, kt = qt_r.next(), kt_r.next()
                s.dve("tensor_tensor", out=qt[:R, :], in0=q[:R, :], in1=ex1[:R, :], op=ALU.mult)
                s.pool("tensor_tensor", out=kt[:R, :], in0=k[:R, :], in1=ex2[:R, :], op=ALU.mult)
                pT = pT_r.next()
                for h in range(4):
                    s.pe("transpose", out=pT[:, h * 128:h * 128 + R], in_=qt[:R, h * 128:(h + 1) * 128], identity=self.ident[:R, :R])
                    s.pe("transpose", out=pT[:, 512 + h * 128:512 + h * 128 + R], in_=kt[:R, h * 128:(h + 1) * 128], identity=self.ident[:R, :R])
                qkT = qkT_r.next()
                s.act("copy", out=qkT[:, :, :R], in_=pT[:, :].rearrange("p (j r) -> p j r", j=8)[:, :, :R])
                pV = pV_r.next()
                for h in range(4):
                    s.pe("matmul", out=pV[:, h * 2:h * 2 + 2], lhsT=g[:R, h * 128:(h + 1) * 128],
                         rhs=K(mv[:R, d, 0 if big else 1, :], mv.name), start=True, stop=True)
                ev = ev_r.next()
                s.act("activation", out=K(ev[:, :, 0:2], ev.name), in_=pV[:, 0:8].rearrange("p (h c) -> p h c", h=4), func=AF.Exp)
                s.dve("tensor_tensor", out=K(ev[:, :, 2:3], ev.name), in0=K(ev[:, :, 0:1], ev.name), in1=K(ev[:, :, 1:2], ev.name), op=ALU.mult)
                s.dve("tensor_tensor", out=Sp[:, :, :], in0=S[:, :, :], in1=K(ev[:, :, 0:1].to_broadcast([128, 4, 128]), ev.name), op=ALU.mult)
                py = py_r.next()
                for h in range(4):
                    pA = pA_r.next()
                    s.pe("matmul", out=pA[:R, :R], lhsT=qkT[:, 4 + h, :R], rhs=qkT[:, h, :R], start=True, stop=True)
                    if big:
                        attS = attS_r.next()
                        if d == 0:
                            s.dve("tensor_tensor", out=K(attS[0:64, :], attS.name), in0=pA[0:64, 0:128], in1=K(M[0:64, :], self.masks.name), op=ALU.mult)
                            s.dve("tensor_tensor", out=K(attS[64:128, 64:128], attS.name), in0=pA[64:128, 64:128], in1=K(M[64:128, 64:128], self.masks.name), op=ALU.mult)
                        else:
                            s.dve("tensor_tensor", out=K(attS[:, 0:64], attS.name), in0=pA[:, 0:64], in1=K(M[:, 0:64], self.masks.name), op=ALU.mult)
                            s.dve("tensor_tensor", out=K(attS[64:128, 64:128], attS.name), in0=pA[64:128, 64:128], in1=K(M[64:128, 64:128], self.masks.name), op=ALU.mult)
                        aT = attS[:, :]
                    else:
                        a16 = att16_r.next()
                        s.dve("tensor_tensor", out=a16[:, :], in0=pA[:R, :R], in1=K(M[:R, :R], self.masks.name), op=ALU.mult)
                        aT = a16[:, :]
                    s.pe("matmul", out=py[:R, h * 128:(h + 1) * 128], lhsT=aT, rhs=v[:R, h * 128:(h + 1) * 128], start=True, stop=False)
                    s.pe("matmul", out=py[:R, h * 128:(h + 1) * 128], lhsT=qkT[:, h, :R], rhs=K(Sp[:, h, :], Sp.name), start=False, stop=True)
                pS = pS_r.next()
                for h in range(4):
                    s.pe("matmul", out=pS[:, h * 128:(h + 1) * 128], lhsT=kt[:R, h * 128:(h + 1) * 128], rhs=v[:R, h * 128:(h + 1) * 128],
                         start=True, stop=True)
                s.dve("tensor_tensor", out=S[:, :, :], in0=S[:, :, :], in1=K(ev[:, :, 2:3].to_broadcast([128, 4, 128]), ev.name), op=ALU.mult)
                s.dve("tensor_tensor", out=Sp[:, :, :], in0=pS[:, :].rearrange("p (h c) -> p h c", h=4),
                      in1=K(ev[:, :, 1:2].to_broadcast([128, 4, 128]), ev.name), op=ALU.mult)
                s.dve("tensor_tensor", out=S[:, :, :], in0=S[:, :, :], in1=Sp[:, :, :], op=ALU.add)
                ot = o_r.next()
                if d == 0:
                    s.act("copy", out=ot[:R, :], in_=py[:R, :])
                    s.dma(STQ, K(self.yh_d[o:o + R, :], ("yh", t)), ot[:R, :])
                else:
                    yf = yf_r.next()
                    s.dma("sp", yf[:R, :], K(self.yh_d[o:o + R, :], ("yh", t)))
                    gt = gt_r.next()
                    s.dma("sp", gt[:R, :], K(self.hgate_d[o:o + R, :], "hgate_all"))
                    s.dve("tensor_tensor", out=ot[:R, :], in0=yf[:R, :], in1=py[:R, :], op=ALU.add)
                    sq = sq_r.next()
                    s.pool("tensor_tensor", out=sq[:R, :], in0=ot[:R, :], in1=ot[:R, :], op=ALU.mult)
                    hs = hs_r.next()
                    s.dve("tensor_reduce", out=hs[:R, :], in_=sq[:R, :].rearrange("p (h c) -> p h c", h=4), axis=AX.X, op=ALU.add)
                    self.rms_rstd(hs[:R, :], R, 128)
                    s.dve("tensor_tensor", out=ot[:R, :].rearrange("p (h c) -> p h c", h=4), in0=ot[:R, :].rearrange("p (h c) -> p h c", h=4),
                          in1=hs[:R, :].unsqueeze(2).to_broadcast([R, 4, 128]), op=ALU.mult)
                    s.pool("tensor_tensor", out=ot[:R, :], in0=ot[:R, :], in1=hnw[:R, :], op=ALU.mult)
                    s.dve("tensor_tensor", out=ot[:R, :], in0=ot[:R, :], in1=gt[:R, :], op=ALU.mult)
                    s.dma(STQ, K(self.mix_d[o:o + R, 512:1024], ("mixB", t)), ot[:R, :])
            s.barrier()
        sc.release(m)

    def phase_D(self, l, li):
        s, sc = self.s, self.sc
        m = sc.mark()
        wo = sc.sb([128, 12, D], BF16, name="wo")
        for k3 in range(3):
            s.dma("pool", K(wo[:, 4 * k3:4 * k3 + 4, :], wo.name),
                  self.w_out[l, 512 * k3:512 * (k3 + 1), :].rearrange("(k p) n -> p k n", p=128))
        w2bc = sc.sb([128, D], name="w2bc")
        s.dma("sp", w2bc[:, :], self.norm2_w[l:l + 1, :].to_broadcast([128, D]))
        rw = sc.sb([128, 8, NEXP], name="rw")
        s.dma("sp", rw[:, :, :], self.router_w[l, :, :].rearrange("(k p) e -> p k e", p=128))
        mix_r = sc.sbring(2, [128, 1536], name="mixt")
        psM = sc.psring(1, [128, 1536], name="psM")
        mixT_r = sc.sbring(2, [128, 12, 128], BF16, name="mixT")
        ph_r = sc.psring(2, [128, 512], name="ph")
        ht_r = sc.sbring(2, [128, D], name="ht")
        hn_r = sc.sbring(2, [128, D], name="hn")
        junk = sc.sb([128, D], name="junkd")
        ssq_r = sc.sbring(2, [128, 1], name="ssqd")
        psH = sc.psring(1, [128, 1024], name="psH")
        hnT_r = sc.sbring(2, [128, 8, 128], name="hnT")
        pm_r = sc.psring(1, [128, 512], name="pm")
        sm_r = sc.sbring(2, [128, 4], name="sm")
        ex_r = sc.sbring(2, [128, NEXP], name="ex")
        for t in range(NT):
            R, o = tile_rows(t), tile_off(t)
            mixt = mix_r.next()
            s.dma("sp", mixt[:R, :], K(self.mix_d[o:o + R, :], "mix_all"))
            pM = psM.next()
            for k in range(12):
                s.pe("transpose", out=pM[:, k * 128:k * 128 + R], in_=mixt[:R, k * 128:(k + 1) * 128], identity=self.ident[:R, :R])
            mixT = mixT_r.next()
            s.act("copy", out=mixT[:, :, :R], in_=pM[:, :].rearrange("p (k r) -> p k r", k=12)[:, :, :R])
            ht = ht_r.next()
            s.dma("sp", ht[:R, :], K(self.h_src(li, t), self.hkey(t)))
            for half in range(2):
                ph = ph_r.next()
                for k in range(12):
                    s.pe("matmul", out=ph[:R, :], lhsT=mixT[:, k, :R], rhs=wo[:, k, half * 512:(half + 1) * 512],
                         start=(k == 0), stop=(k == 11))
                s.dve("tensor_tensor", out=K(ht[:R, half * 512:(half + 1) * 512], ht.name),
                      in0=K(ht[:R, half * 512:(half + 1) * 512], ht.name), in1=ph[:R, :], op=ALU.add)
            s.dma(STQ, K(self.y[o:o + R, :], self.hkey(t)), ht[:R, :])
            ssq = ssq_r.next()
            s.act("activation", out=junk[:R, :], in_=ht[:R, :], func=AF.Square, accum_out=ssq[:R, 0:1])
            self.rms_rstd(ssq[:R, 0:1], R, D)
            hn = hn_r.next()
            s.dve("scalar_tensor_tensor", out=hn[:R, :], in0=ht[:R, :], scalar=ssq[:R, 0:1], in1=w2bc[:R, :],
                  op0=ALU.mult, op1=ALU.mult)
            s.dma(STQ, K(self.hn_d[o:o + R, :], ("hn", t)), hn[:R, :])
            pH = psH.next()
            for k in range(8):
                s.pe("transpose", out=pH[:, k * 128:k * 128 + R], in_=hn[:R, k * 128:(k + 1) * 128], identity=self.ident[:R, :R])
            hnT = hnT_r.next()
            s.act("copy", out=hnT[:, :, :R], in_=pH[:, :].rearrange("p (k r) -> p k r", k=8)[:, :, :R])
            pm = pm_r.next()
            for k in range(8):
                s.pe("matmul", out=pm[:R, 0:NEXP], lhsT=hnT[:, k, :R], rhs=rw[:, k, :], start=(k == 0), stop=(k == 7))
            sm = sm_r.next()
            ex = ex_r.next()
            s.dve("tensor_reduce", out=K(sm[:R, 0:1], sm.name), in_=pm[:R, 0:NEXP], axis=AX.X, op=ALU.max)
            s.dve("tensor_scalar", out=K(sm[:R, 1:2], sm.name), in0=K(sm[:R, 0:1], sm.name), scalar1=-1.0, scalar2=None, op0=ALU.mult)
            s.act("activation", out=ex[:R, :], in_=pm[:R, 0:NEXP], func=AF.Exp, bias=K(sm[:R, 1:2], sm.name),
                  accum_out=K(sm[:R, 2:3], sm.name))
            s.dve("reciprocal", out=K(sm[:R, 3:4], sm.name), in_=K(sm[:R, 2:3], sm.name))
            s.dve("tensor_scalar", out=ex[:R, :], in0=ex[:R, :], scalar1=K(sm[:R, 3:4], sm.name), scalar2=None, op0=ALU.mult)
            s.pe("transpose", out=pm[:NEXP, 128:128 + R], in_=ex[:R, :], identity=self.ident[:R, :R])
            s.act("copy", out=K(self.affT[:, o:o + R], self.affT.name), in_=pm[:NEXP, 128:128 + R])
        s.barrier()
        sc.release(m)

    def phase_E(self, l, li):
        s, sc = self.s, self.sc
        m = sc.mark()
        NI = 65
        work = sc.sb([16, L], name="work")
        vals = sc.sb([16, NI * 8], name="vals")
        idxs = sc.sb([16, NI * 8], U32, name="idxs")
        s.dve("tensor_copy", out=work[:, :], in_=self.affT[:, :])
        for i in range(NI):
            v8 = K(vals[:, 8 * i:8 * i + 8], ("vals", i))
            s.dve("max", out=v8, in_=work[:, :])
            s.dve("max_index", out=K(idxs[:, 8 * i:8 * i + 8], ("idxs", i)), in_max=v8, in_values=work[:, :])
            s.dve("match_replace", out=work[:, :], in_to_replace=v8, in_values=work[:, :], imm_value=-1.0)
        s.dma(STQ, K(self.gate_d[:, :], "gate_d"), K(vals[:, :], "valsall"), _r=[("vals", i) for i in range(NI)])
        s.dma(STQ, K(self.idx_d[:, :], "idx_d"), K(idxs[:, :].bitcast(I32), "idxsall"), _r=[("idxs", i) for i in range(NI)])
        s.barrier()
        sc.release(m)

    def phase_F(self, l, li):
        s, sc = self.s, self.sc
        m = sc.mark()
        tiles = [(c * 128, 128) for c in range(4)] + [(512, CAP - 512)]
        ix_r = sc.sbring(2, [128, 5], I32, name="ix")
        gt_r = sc.sbring(2, [128, 5], name="gt")
        xs_r = sc.sbring(2, [128, D], name="xs")
        psX = sc.psring(1, [128, 1024], name="psX")
        xsT_r = sc.sbring(1, [128, 8, 640], BF16, name="xsT")
        wg_r = sc.sbring(2, [128, 8, 512], BF16, name="wg")
        wu_r = sc.sbring(2, [128, 8, 512], BF16, name="wu")
        wd_r = sc.sbring(2, [128, 4, D], BF16, name="wd")
        pA_r = sc.psring(1, [128, 512], name="pA")
        pU_r = sc.psring(1, [128, 512], name="pU")
        pS_r = sc.psring(1, [128, 512], name="pS")
        pD_r = sc.psring(3, [128, 512], name="pD")
        sl_r = sc.sbring(2, [128, 512], name="sl")
        hT_r = sc.sbring(2, [128, 4, 640], BF16, name="hT")
        yacc_r = sc.sbring(1, [128, 5, D], name="yacc")
        for e in range(NEXP):
            ix = ix_r.next()
            gt = gt_r.next()
            s.dma("sp", K(ix[:, 0:4], ix.name), K(self.idx_d[e, 0:512].rearrange("(c p) -> p c", p=128), "idx_d"),
                  allow_slow_non_contiguous=True)
            s.dma("sp", K(ix[0:2, 4:5], ix.name), K(self.idx_d[e:e + 1, 512:514].rearrange("o p -> p o"), "idx_d"),
                  allow_slow_non_contiguous=True)
            s.dma("sp", K(gt[:, 0:4], gt.name), K(self.gate_d[e, 0:512].rearrange("(c p) -> p c", p=128), "gate_d"),
                  allow_slow_non_contiguous=True)
            s.dma("sp", K(gt[0:2, 4:5], gt.name), K(self.gate_d[e:e + 1, 512:514].rearrange("o p -> p o"), "gate_d"),
                  allow_slow_non_contiguous=True)
            xsT = xsT_r.next()
            for c, (co, n) in enumerate(tiles):
                xs = xs_r.next()
                s.dma("pool", xs[:n, :], K(self.hn_d[:, :], "hn_all"), indirect=True,
                      _r=[ix.name], out_offset=None,
                      in_offset=bass.IndirectOffsetOnAxis(ap=ix[:n, c:c + 1], axis=0))
                pX = psX.next()
                for k in range(8):
                    s.pe("transpose", out=pX[:, k * 128:k * 128 + n], in_=xs[:n, k * 128:(k + 1) * 128], identity=self.ident[:n, :n])
                s.act("copy", out=K(xsT[:, :, co:co + n], xsT.name), in_=pX[:, :].rearrange("p (k r) -> p k r", k=8)[:, :, :n])
            yacc = yacc_r.next()
            for gi in range(4):
                wg, wu, wd = wg_r.next(), wu_r.next(), wd_r.next()
                s.dma("poolw", wg[:, :, :], self.w_gate[l, e, :, gi * 512:(gi + 1) * 512].rearrange("(k p) f -> p k f", p=128))
                s.dma("poolw", wu[:, :, :], self.w_up[l, e, :, gi * 512:(gi + 1) * 512].rearrange("(k p) f -> p k f", p=128))
                s.dma("poolw", wd[:, :, :], self.w_down[l, e, gi * 512:(gi + 1) * 512, :].rearrange("(c p) d -> p c d", p=128))
                hT = hT_r.next()
                for c4 in range(4):
                    pA, pU, pS = pA_r.next(), pU_r.next(), pS_r.next()
                    for (w_, p_, so) in ((wg, pA, 0), (wu, pU, 8)):
                        for k in range(8):
                            s.pe("matmul", out=p_[:, :], lhsT=w_[:, k, c4 * 128:(c4 + 1) * 128], rhs=xsT[:, k, 0:512],
                                 start=(k == 0), stop=(k == 7))
                        for k in range(8):
                            s.pe("matmul", out=pS[:, so:so + 2], lhsT=w_[:, k, c4 * 128:(c4 + 1) * 128], rhs=xsT[:, k, 512:514],
                                 start=(k == 0), stop=(k == 7))
                    sl = sl_r.next()
                    s.act("activation", out=sl[:, :], in_=pA[:, :], func=AF.Silu)
                    s.dve("tensor_tensor", out=K(hT[:, c4, 0:512], hT.name), in0=sl[:, :], in1=pU[:, :], op=ALU.mult)
                    sl2 = sl_r.next()
                    s.act("activation", out=sl2[:, 0:2], in_=pS[:, 0:2], func=AF.Silu)
                    s.dve("tensor_tensor", out=K(hT[:, c4, 512:514], hT.name), in0=sl2[:, 0:2], in1=pS[:, 8:10], op=ALU.mult)
                for c, (co, n) in enumerate(tiles):
                    for half in range(2):
                        pD = pD_r.next()
                        for c4 in range(4):
                            s.pe("matmul", out=pD[:n, :], lhsT=hT[:, c4, co:co + n], rhs=wd[:, c4, half * 512:(half + 1) * 512],
                                 start=(c4 == 0), stop=(c4 == 3))
                        ya = K(yacc[:n, c, half * 512:(half + 1) * 512], (yacc.name, c))
                        if gi == 0:
                            s.act("copy", out=ya, in_=pD[:n, :])
                        else:
                            s.dve("tensor_tensor", out=ya, in0=ya, in1=pD[:n, :], op=ALU.add)
            for c, (co, n) in enumerate(tiles):
                ya = K(yacc[:n, c, :], (yacc.name, c))
                s.dve("tensor_scalar", out=ya, in0=ya, scalar1=K(gt[:n, c:c + 1], gt.name), scalar2=None, op0=ALU.mult)
                s.dma("pool", K(self.y[:, :], "y_acc"), ya, indirect=True, _r=[ix.name],
                      out_offset=bass.IndirectOffsetOnAxis(ap=ix[:n, c:c + 1], axis=0), in_offset=None,
                      compute_op=ALU.add)
        s.barrier()
        sc.release(m)

    def build(self):
        s = self.s
        self.load_consts()
        for li, l in enumerate(self.layers):
            ph = self.phases
            if ph == "all" or "A1" in ph:
                self.phase_A1(l, li)
            if ph == "all" or "B1" in ph:
                self.phase_B1(l, li)
            if ph == "all" or "A2" in ph:
                self.phase_A2(l, li)
            if ph == "all" or "B2" in ph:
                self.phase_B2(l, li)
            if ph == "all" or "A3" in ph:
                self.phase_A3(l, li)
            if ph == "all" or "B3a" in ph:
                self.phase_B3a(l, li)
            if ph == "all" or "B3b" in ph:
                self.phase_B3b(l, li)
            if "MIXIN" in ph:
                mixin = self.nc.dram_tensor("mixin", [L, 1536], F32, kind="ExternalInput").ap()
                self._in["mixin"] = mixin
                s.dma("sp", K(self.mix_d[:, :], "mix_all"), K(mixin[:, :], "mixin"))
                s.barrier()
            if ph == "all" or "D" in ph:
                self.phase_D(l, li)
            if ph == "all" or "E" in ph:
                self.phase_E(l, li)
            if ph == "all" or "F" in ph:
                self.phase_F(l, li)
        for name in self.debug:
            src = getattr(self, name)
            dst = self.nc.dram_tensor("dbg_" + name, list(src.shape), F32, kind="ExternalOutput").ap()
            s.dma("sp", K(dst, "dbgout"), K(src, "dbgsrc"))
        s.finish()
        return self.nc


def host_consts():
    ident = np.eye(128, dtype=np.float32)
    n_pair = 16
    inv = (10000.0 ** (-np.arange(n_pair, dtype=np.float32) / n_pair)).astype(np.float32)
    row = np.repeat(np.arange(SEQ // 64), 64).astype(np.float32)
    col = (np.arange(SEQ) % 64).astype(np.float32)
    ang = np.concatenate([row[:, None] * inv, col[:, None] * inv], axis=-1)
    ang = np.concatenate([np.zeros((NMETA, 32), np.float32), ang], axis=0).astype(np.float32)
    cs = np.concatenate([np.cos(ang), np.sin(ang)], axis=-1).astype(np.float32)
    masks = np.zeros((128, 8, 128), np.float32)
    jj, ii = np.meshgrid(np.arange(128), np.arange(128), indexing="ij")
    masks[:, 0, :] = (jj <= ii)
    masks[:, 1, :] = (jj >= ii)
    masks[:, 2, :] = np.where(jj <= ii, 0.0, -30000.0)
    masks[:, 3, :] = np.where(jj >= ii, 0.0, -30000.0)
    masks[:, 4, :] = (jj <= ii).astype(np.float32) - (jj <= 63)
    masks[:, 5, :] = (jj >= ii).astype(np.float32) - (jj >= 64)
    masks[:, 6, :] = (jj <= ii).astype(np.float32) - 1.0
    masks[:, 7, :] = (jj >= ii).astype(np.float32) - 1.0
    return dict(c_ident=ident, c_cs=cs, c_masks=masks)


PARAM_NAMES = ["meta_tokens", "norm1_w", "w_in", "q_norm_w", "k_norm_w", "attn_norm_w", "hgrn_lb", "hgrn_norm_w",
               "conv_w", "conv_b", "dt_bias", "a_log", "d_skip", "ssm_norm_w", "w_out", "norm2_w", "router_w",
               "w_gate", "w_up", "w_down"]


def make_in_maps(inputs, cores, used=None):
    consts = host_consts()
    shared = {}
    for n in PARAM_NAMES:
        a = np.ascontiguousarray(inputs[n], dtype=np.float32)
        if n in ("dt_bias", "a_log"):
            a = a.reshape(DEPTH, 16)
        shared[n] = a
    shared.update(consts)
    maps = []
    for c in cores:
        m = dict(shared)
        m["x"] = np.ascontiguousarray(inputs["x"][c], dtype=np.float32)
        if used is not None:
            m = {k: v for k, v in m.items() if k in used}
        maps.append(m)
    return maps


def kernel(**inputs):
    mk = MK()
    nc = mk.build()
    maps = make_in_maps(inputs, list(range(8)), used=set(mk._in))
    res = run_bass_kernel_spmd(nc, maps, core_ids=list(range(8)))
    out = np.stack([np.asarray(r["y"])[NMETA:] for r in res.results], axis=0)
    return out.astype(np.float32)
```
